# Optimizing a Trainium2 kernel written in Bass

```python
import math
import jax, jax.numpy as jnp
from jax import lax
import numpy as np

D_MODEL = 1024
BATCH = 4
SEQ = 8192
DEPTH = 4

N_MIXERS = 2
N_SELF_HEADS = 12
N_MEM_HEADS = 4
HEAD_DIM = 64
SELF_WIDTH = N_SELF_HEADS * HEAD_DIM
MEM_WIDTH = N_MEM_HEADS * HEAD_DIM
MIX_WIDTH = SELF_WIDTH + MEM_WIDTH
N_MEM = 256
D_FF = -(-8 * D_MODEL // (3 * 256)) * 256
FOX_Q_BLOCK = 128
MOBA_BLOCK = 256
MOBA_TOPK = 3
MOBA_Q_CHUNK = 32
N_FOX_LAYERS = (DEPTH + 1) // 2
N_MOBA_LAYERS = DEPTH // 2
FOX_PROJ = 3 * SELF_WIDTH + N_SELF_HEADS + MEM_WIDTH
MOBA_PROJ = 3 * SELF_WIDTH + MEM_WIDTH
NEG = -1e30
RMS_EPS = 1e-6

kernel_name = "hybrid_fox_moba_memory_trunk"


def rms_norm(x, g):
    xf = x.astype(jnp.float32)
    y = xf * lax.rsqrt(jnp.mean(xf * xf, axis=-1, keepdims=True) + RMS_EPS)
    return (y * g.astype(jnp.float32)).astype(x.dtype)


def split_heads(t, n_heads):
    b, s, _ = t.shape
    return t.reshape(b, s, n_heads, HEAD_DIM).transpose(0, 2, 1, 3)


def merge_heads(t):
    b, h, s, d = t.shape
    return t.transpose(0, 2, 1, 3).reshape(b, s, h * d)


def alibi_slopes(n_heads):
    return 2.0 ** (-8.0 * jnp.arange(1, n_heads + 1, dtype=jnp.float32) / n_heads)


def fox_attention(q, k, v, log_f):
    B, H, S, Dh = q.shape
    F = jnp.cumsum(log_f, axis=-1)
    scale = Dh ** -0.5
    kpos = jnp.arange(S)

    def one_block(i):
        start = i * FOX_Q_BLOCK
        qb = lax.dynamic_slice_in_dim(q, start, FOX_Q_BLOCK, axis=2)
        Fq = lax.dynamic_slice_in_dim(F, start, FOX_Q_BLOCK, axis=2)
        tq = start + jnp.arange(FOX_Q_BLOCK)
        s = (jnp.einsum('bhqd,bhkd->bhqk', qb, k).astype(jnp.float32) * scale
             + Fq[..., :, None] - F[..., None, :])
        s = jnp.where(tq[:, None] >= kpos[None, :], s, NEG)
        p = jax.nn.softmax(s, axis=-1)
        return jnp.einsum('bhqk,bhkd->bhqd', p.astype(v.dtype), v)

    out = lax.map(one_block, jnp.arange(S // FOX_Q_BLOCK))
    return out.transpose(1, 2, 0, 3, 4).reshape(B, H, S, Dh)


def moba_attention(q, k, v, slopes):
    B, H, S, Dh = q.shape
    L = MOBA_BLOCK
    nb = -(-S // L)
    S_pad = nb * L
    K = min(MOBA_TOPK, nb)
    pad = [(0, 0), (0, 0), (0, S_pad - S), (0, 0)]
    qp, kp, vp = jnp.pad(q, pad), jnp.pad(k, pad), jnp.pad(v, pad)
    kb = kp.reshape(B, H, nb, L, Dh)
    vb = vp.reshape(B, H, nb, L, Dh)
    kmean = jnp.mean(kb.astype(jnp.float32), axis=3)
    scale = Dh ** -0.5
    blk_ids = jnp.arange(nb)
    bi = jnp.arange(B)[:, None, None, None]
    hi = jnp.arange(H)[None, :, None, None]
    sl = slopes[None, :, None, None]

    def one_chunk(c):
        start = c * MOBA_Q_CHUNK
        qc = lax.dynamic_slice_in_dim(qp, start, MOBA_Q_CHUNK, axis=2)
        tq = start + jnp.arange(MOBA_Q_CHUNK)
        own = start // L
        gate = jnp.einsum('bhqd,bhnd->bhqn', qc.astype(jnp.float32), kmean)
        gate = jnp.where(blk_ids < own, gate, NEG)
        _, idx = lax.top_k(gate, K)
        valid = idx < own
        ksel = kb[bi, hi, idx]
        vsel = vb[bi, hi, idx]
        s_sel = jnp.einsum('bhqd,bhqkld->bhqkl', qc, ksel).astype(jnp.float32) * scale
        kpos_sel = idx[..., None] * L + jnp.arange(L)
        dist_sel = (tq[None, None, :, None, None] - kpos_sel).astype(jnp.float32)
        s_sel = jnp.where(valid[..., None], s_sel - sl[..., None] * dist_sel, NEG)
        s_sel = s_sel.reshape(B, H, MOBA_Q_CHUNK, K * L)
        kown = lax.dynamic_slice_in_dim(kp, own * L, L, axis=2)
        vown = lax.dynamic_slice_in_dim(vp, own * L, L, axis=2)
        opos = own * L + jnp.arange(L)
        dist_own = (tq[:, None] - opos[None, :]).astype(jnp.float32)
        s_own = (jnp.einsum('bhqd,bhld->bhql', qc, kown).astype(jnp.float32) * scale
                 - sl * dist_own)
        s_own = jnp.where(dist_own >= 0, s_own, NEG)
        p = jax.nn.softmax(jnp.concatenate([s_sel, s_own], axis=-1), axis=-1)
        p_sel = p[..., :K * L].astype(v.dtype)
        p_own = p[..., K * L:].astype(v.dtype)
        vsel = vsel.reshape(B, H, MOBA_Q_CHUNK, K * L, Dh)
        return (jnp.einsum('bhqm,bhqmd->bhqd', p_sel, vsel)
                + jnp.einsum('bhql,bhld->bhqd', p_own, vown))

    out = lax.map(one_chunk, jnp.arange(S_pad // MOBA_Q_CHUNK))
    out = out.transpose(1, 2, 0, 3, 4).reshape(B, H, S_pad, Dh)
    return out[:, :, :S]


def memory_attention(qm, mk, mv):
    s = jnp.einsum('bhsd,bhnd->bhsn', qm, mk).astype(jnp.float32) * (HEAD_DIM ** -0.5)
    p = jax.nn.softmax(s, axis=-1)
    return jnp.einsum('bhsn,bhnd->bhsd', p.astype(mv.dtype), mv)


def setup_inputs(seed: int = 0) -> dict:
    key = jax.random.key(seed)
    ks = jax.random.split(key, 14)
    f32 = jnp.float32
    x = jax.random.normal(ks[0], (BATCH, SEQ, D_MODEL), f32)
    mem = jax.random.normal(ks[1], (BATCH, N_MEM, D_MODEL), f32)
    norm_mix = 1.0 + 0.02 * jax.random.normal(ks[2], (DEPTH, D_MODEL), f32)
    norm_mem = 1.0 + 0.02 * jax.random.normal(ks[3], (DEPTH, D_MODEL), f32)
    norm_ffn = 1.0 + 0.02 * jax.random.normal(ks[4], (DEPTH, D_MODEL), f32)
    norm_final = 1.0 + 0.02 * jax.random.normal(ks[5], (D_MODEL,), f32)
    w_in_fox = jax.random.normal(ks[6], (N_FOX_LAYERS, D_MODEL, FOX_PROJ), f32) * D_MODEL ** -0.5
    b_fgate = jax.random.uniform(ks[7], (N_FOX_LAYERS, N_SELF_HEADS), f32, 1.0, 4.0)
    w_in_moba = jax.random.normal(ks[8], (N_MOBA_LAYERS, D_MODEL, MOBA_PROJ), f32) * D_MODEL ** -0.5
    w_mem_kv = jax.random.normal(ks[9], (DEPTH, D_MODEL, 2 * MEM_WIDTH), f32) * D_MODEL ** -0.5
    w_out = jax.random.normal(ks[10], (DEPTH, MIX_WIDTH, D_MODEL), f32) * MIX_WIDTH ** -0.5
    w_gate_up = jax.random.normal(ks[11], (DEPTH, D_MODEL, 2 * D_FF), f32) * D_MODEL ** -0.5
    w_down = jax.random.normal(ks[12], (DEPTH, D_FF, D_MODEL), f32) * D_FF ** -0.5
    return {"x": x, "mem": mem, "norm_mix": norm_mix, "norm_mem": norm_mem,
            "norm_ffn": norm_ffn, "norm_final": norm_final, "w_in_fox": w_in_fox,
            "b_fgate": b_fgate, "w_in_moba": w_in_moba, "w_mem_kv": w_mem_kv,
            "w_out": w_out, "w_gate_up": w_gate_up, "w_down": w_down}


def reference(x, mem, norm_mix, norm_mem, norm_ffn, norm_final, w_in_fox, b_fgate,
              w_in_moba, w_mem_kv, w_out, w_gate_up, w_down):
    slopes = alibi_slopes(N_SELF_HEADS)
    h = x
    for i in range(DEPTH):
        xn = rms_norm(h, norm_mix[i])
        mn = rms_norm(mem, norm_mem[i])
        mkv = mn @ w_mem_kv[i]
        mk = split_heads(mkv[..., :MEM_WIDTH], N_MEM_HEADS)
        mv = split_heads(mkv[..., MEM_WIDTH:], N_MEM_HEADS)
        j = i // N_MIXERS
        if i % N_MIXERS == 0:
            proj = xn @ w_in_fox[j]
            q = split_heads(proj[..., :SELF_WIDTH], N_SELF_HEADS)
            k = split_heads(proj[..., SELF_WIDTH:2 * SELF_WIDTH], N_SELF_HEADS)
            v = split_heads(proj[..., 2 * SELF_WIDTH:3 * SELF_WIDTH], N_SELF_HEADS)
            f_logit = proj[..., 3 * SELF_WIDTH:3 * SELF_WIDTH + N_SELF_HEADS]
            qm = split_heads(proj[..., 3 * SELF_WIDTH + N_SELF_HEADS:], N_MEM_HEADS)
            log_f = jax.nn.log_sigmoid(f_logit.astype(jnp.float32)
                                       + b_fgate[j].astype(jnp.float32))
            self_out = fox_attention(q, k, v, log_f.transpose(0, 2, 1))
        else:
            proj = xn @ w_in_moba[j]
            q = split_heads(proj[..., :SELF_WIDTH], N_SELF_HEADS)
            k = split_heads(proj[..., SELF_WIDTH:2 * SELF_WIDTH], N_SELF_HEADS)
            v = split_heads(proj[..., 2 * SELF_WIDTH:3 * SELF_WIDTH], N_SELF_HEADS)
            qm = split_heads(proj[..., 3 * SELF_WIDTH:], N_MEM_HEADS)
            self_out = moba_attention(q, k, v, slopes)
        mem_out = memory_attention(qm, mk, mv)
        heads = jnp.concatenate([merge_heads(self_out), merge_heads(mem_out)], axis=-1)
        h = h + heads @ w_out[i]
        hn = rms_norm(h, norm_ffn[i])
        gu = hn @ w_gate_up[i]
        h = h + (jax.nn.silu(gu[..., :D_FF]) * gu[..., D_FF:]) @ w_down[i]
    return rms_norm(h, norm_final)
```

```python
import numpy as np
import ml_dtypes
from contextlib import ExitStack
import concourse.bass as bass
import concourse.mybir as mybir
from concourse.bass_utils import run_bass_kernel_spmd

F32 = mybir.dt.float32
BF16 = mybir.dt.bfloat16
ALU = mybir.AluOpType
AF = mybir.ActivationFunctionType
AX = mybir.AxisListType

D = 1024
NH = 12
NMH = 4
HD = 64
SW = 768
MW = 256
NMEM = 256
DFF = 2816
FOXP = 2572
MOBAP = 2560
EPS = 1e-6
MASKV = -29952.0
NC8 = 8

COMPUTE = ("pe", "act", "dve", "pool")


class Buf:
    __slots__ = ("name", "lw", "rd")

    def __init__(self, name=""):
        self.name = name
        self.lw = None
        self.rd = []


class DmaSem:
    __slots__ = ("sem", "n")

    def __init__(self, sem):
        self.sem = sem
        self.n = 0


class Sched:
    def __init__(self, nc, stack):
        self.nc = nc
        self.stack = stack
        self.q = {e: [] for e in ("pe", "act", "dve", "pool", "sp")}
        self.psem = {}
        self.cnt = {}
        for e in COMPUTE:
            self.psem[e] = stack.enter_context(nc.semaphore("prog_" + e))
            self.cnt[e] = 0
        self.waited = {e: {} for e in self.q}
        self.dsems = []
        self.ninstr = 0

    def dmasem(self, name):
        s = self.stack.enter_context(self.nc.semaphore("d_" + name))
        d = DmaSem(s)
        self.dsems.append(d)
        return d

    def _deps(self, eng, reads, writes):
        deps = []
        for r in reads:
            if r.lw is not None:
                deps.append(r.lw)
        for w in writes:
            if w.lw is not None:
                deps.append(w.lw)
            deps.extend(w.rd)
        need = {}
        for (sem, val, teng) in deps:
            if eng == "pe" and teng == "pe":
                continue
            k = id(sem)
            if k not in need or need[k][1] < val:
                need[k] = (sem, val)
        waits = []
        wd = self.waited[eng]
        for k, (sem, val) in need.items():
            if wd.get(k, 0) >= val:
                continue
            wd[k] = val
            waits.append((sem, val))
        return waits

    @staticmethod
    def _commit(tok, reads, writes):
        for r in reads:
            r.rd.append(tok)
        for w in writes:
            w.lw = tok
            w.rd = []

    def op(self, eng, fn, reads=(), writes=()):
        waits = self._deps(eng, reads, writes)
        self.cnt[eng] += 1
        tok = (self.psem[eng], self.cnt[eng], eng)
        self.q[eng].append((waits, fn, (self.psem[eng], 1)))
        self._commit(tok, reads, writes)
        self.ninstr += 1
        return tok

    def dma_group(self, q, dsem, items):
        for fn, reads, writes in items:
            waits = self._deps(q, reads, writes)
            dsem.n += 1
            self.q[q].append((waits, fn, (dsem.sem, 16)))
            self.ninstr += 1
        tok = (dsem.sem, 16 * dsem.n, "dma")
        for fn, reads, writes in items:
            self._commit(tok, reads, writes)
        return tok

    def dma(self, q, dsem, fn, reads=(), writes=()):
        return self.dma_group(q, dsem, [(fn, reads, writes)])

    def barrier(self):
        for e in self.q:
            waits = []
            wd = self.waited[e]
            for o in COMPUTE:
                if o == e or self.cnt[o] == 0:
                    continue
                k = id(self.psem[o])
                if wd.get(k, 0) < self.cnt[o]:
                    wd[k] = self.cnt[o]
                    waits.append((self.psem[o], self.cnt[o]))
            for d in self.dsems:
                if d.n == 0:
                    continue
                k = id(d.sem)
                if wd.get(k, 0) < 16 * d.n:
                    wd[k] = 16 * d.n
                    waits.append((d.sem, 16 * d.n))
            if waits:
                self.q[e].append((waits, None, None))

    def emit(self):
        nc = self.nc
        q = self.q

        def run(engobj, lst):
            for waits, fn, inc in lst:
                for sem, val in waits:
                    engobj.wait_ge(sem, val)
                if fn is not None:
                    ins = fn(engobj)
                    ins.then_inc(inc[0], inc[1])

        with nc.Block() as block:
            @block.sync
            def _(e):
                run(e, q["sp"])

            @block.tensor
            def _(e):
                run(e, q["pe"])

            @block.scalar
            def _(e):
                run(e, q["act"])

            @block.vector
            def _(e):
                run(e, q["dve"])

            @block.gpsimd
            def _(e):
                run(e, q["pool"])


class Prog:
    def __init__(self, S_len, depth):
        self.SL = S_len
        self.depth = depth
        self.NT = S_len // 128
        self.NQ = S_len // 512
        self.NB = S_len // 256

    def mm(self, out, lhsT, rhs, start, stop, reads, writes):
        return self.S.op("pe", lambda e: e.matmul(out, lhsT=lhsT, rhs=rhs, start=start, stop=stop), reads, writes)

    def act(self, out, in_, func, reads, writes, bias=None, scale=None, eng="act"):
        kw = {}
        if bias is not None:
            kw["bias"] = bias
        if scale is not None:
            kw["scale"] = scale
        return self.S.op("act", lambda e: e.activation(out=out, in_=in_, func=func, **kw), reads, writes)

    def copy(self, eng, out, in_, reads, writes):
        if eng == "act":
            return self.S.op("act", lambda e: e.copy(out=out, in_=in_), reads, writes)
        return self.S.op(eng, lambda e: e.tensor_copy(out=out, in_=in_), reads, writes)

    def tt(self, eng, out, in0, in1, op, reads, writes):
        return self.S.op(eng, lambda e: e.tensor_tensor(out=out, in0=in0, in1=in1, op=op), reads, writes)

    def ts(self, eng, out, in0, s1, s2, op0, op1, reads, writes):
        if op1 is None:
            return self.S.op(eng, lambda e: e.tensor_scalar(out=out, in0=in0, scalar1=s1, scalar2=None, op0=op0), reads, writes)
        return self.S.op(eng, lambda e: e.tensor_scalar(out=out, in0=in0, scalar1=s1, scalar2=s2, op0=op0, op1=op1), reads, writes)

    def memset(self, eng, ap, val, writes):
        return self.S.op(eng, lambda e: e.memset(ap, val), (), writes)

    def dmaf(self, out, in_):
        return lambda e: e.dma_start(out=out, in_=in_)

    def sb(self, shape, dt, region):
        nbytes = int(np.prod(shape[1:])) * (4 if dt == F32 else 2)
        nbytes = (nbytes + 63) // 64 * 64
        off = self.off[region]
        self.off[region] = off + nbytes
        assert self.off[region] <= self.lim[region], (region, self.off[region], self.lim[region])
        self.nt += 1
        return self.nc.alloc_sbuf_tensor_at("t%d" % self.nt, list(shape), dt, offset=off)

    def reset_region(self, region, base, lim):
        self.off[region] = base
        self.lim[region] = lim

    def build(self):
        SL, NT, NQ, NB = self.SL, self.NT, self.NQ, self.NB
        nc = bass.Bass("TRN2", target_bir_lowering=False)
        self.nc = nc
        dt_in = lambda name, shape, dt=F32: nc.dram_tensor(name, list(shape), dt, kind="ExternalInput").ap()
        dscr = lambda name, shape, dt: nc.dram_tensor(name, list(shape), dt, kind="Internal").ap()
        L = self.depth
        NF = (L + 1) // 2
        NM = L // 2
        self.xT = dt_in("xT", [D, SL])
        self.memT = dt_in("memT", [D, NMEM])
        self.g_mix = dt_in("g_mix", [128, L * 8])
        self.g_mem = dt_in("g_mem", [128, L * 8])
        self.g_ffn = dt_in("g_ffn", [128, L * 8])
        self.g_fin = dt_in("g_fin", [128, 8])
        self.w_fox = dt_in("w_in_fox", [NF, D, FOXP])
        self.bfg = dt_in("b_fgate", [NH, NF])
        self.w_moba = dt_in("w_in_moba", [max(NM, 1), D, MOBAP])
        self.w_mkv = dt_in("w_mem_kv", [L, D, 2 * MW])
        self.w_out = dt_in("w_out", [L, D, D])
        self.w_gu = dt_in("w_gate_up", [L, D, 2 * DFF])
        self.w_dn = dt_in("w_down", [L, DFF, D])
        self.c_identb = dt_in("c_identb", [128, 128], BF16)
        self.c_identf = dt_in("c_identf", [128, 128])
        self.c_tri = dt_in("c_tri", [128, 128], BF16)
        self.c_onehot = dt_in("c_onehot", [32, SL], BF16)
        self.c_akb = dt_in("c_alibi_kbias", [128, NH * NT])
        self.c_aqs = dt_in("c_alibi_qshift", [NH, SL], BF16)
        self.c_bm = dt_in("c_moba_biasmask", [128, NB * 32])
        self.outT = nc.dram_tensor("outT", [D, SL], F32, kind="ExternalOutput").ap()
        self.hT = dscr("hT", [D, SL], F32)
        self.qT = dscr("qT", [SW, SL], BF16)
        self.kT = dscr("kT", [SW, SL], BF16)
        self.vv = dscr("vv", [SL, SW], BF16)
        self.qmT = dscr("qmT", [MW, SL], BF16)
        self.mkT = dscr("mkT", [MW, NMEM], BF16)
        self.mv = dscr("mv", [NMEM, MW], BF16)
        self.hdT = dscr("hdT", [D, SL], BF16)
        self.g8 = dscr("g8", [NH, SL], BF16)

        with ExitStack() as st:
            S = Sched(nc, st)
            self.S = S
            self.nt = 0
            self.off = {}
            self.lim = {}
            TOTAL = 229376
            self.reset_region("P", 16640, 16640 + 12800)
            PB = 16640 + 12800
            P = self
            self.identb = self.sb([128, 128], BF16, "P")
            self.identf = self.sb([128, 128], F32, "P")
            self.tri = self.sb([128, 128], BF16, "P")
            self.onesm = self.sb([128, 128], BF16, "P")
            self.onesf = self.sb([128, 64], F32, "P")
            self.gmix = self.sb([128, L * 8], F32, "P")
            self.gmem = self.sb([128, L * 8], F32, "P")
            self.gffn = self.sb([128, L * 8], F32, "P")
            self.gfin = self.sb([128, 8], F32, "P")
            self.nbfg = self.sb([NH, NF], F32, "P")
            self.akb = self.sb([128, NH * NT], F32, "P")
            self.bm = self.sb([128, NB * 32], F32, "P")
            self.gtab = self.sb([128, NH * NT], F32, "P")
            self.epsb = self.sb([128, 1], F32, "P")
            self.B_const = Buf("const")
            self.B_gtab = Buf("gtab")
            csem = S.dmasem("const")
            items = []
            for dst, src in ((self.identb, self.c_identb), (self.identf, self.c_identf), (self.tri, self.c_tri),
                             (self.gmix, self.g_mix), (self.gmem, self.g_mem), (self.gffn, self.g_ffn),
                             (self.gfin, self.g_fin), (self.nbfg, self.bfg), (self.akb, self.c_akb),
                             (self.bm, self.c_bm)):
                items.append((self.dmaf(dst[:], src), (), (self.B_const,)))
            S.dma_group("sp", csem, items)
            self.memset("dve", self.onesm[:], 1.0 / 1024.0, (self.B_const,))
            self.memset("dve", self.onesf[:], 1.0, (self.B_const,))
            self.memset("dve", self.epsb[:], EPS, (self.B_const,))
            self.ts("dve", self.nbfg[:], self.nbfg[:], -1.0, None, ALU.mult, None, (self.B_const,), (self.B_const,))
            self.ps = [st.enter_context(nc.psum_tensor("ps%d" % i, [128, 512], F32)) for i in range(8)]
            self.B_ps = [Buf("ps%d" % i) for i in range(8)]
            self.dsem_ld = [S.dmasem("ld%d" % i) for i in range(4)]
            self.dsem_st = [S.dmasem("st%d" % i) for i in range(6)]
            self.dsem_w = [S.dmasem("w%d" % i) for i in range(3)]
            self.dsem_misc = S.dmasem("misc")
            self.dsem_misc2 = S.dmasem("misc2")
            self.B_dram = {k: Buf(k) for k in ("hT", "qT", "kT", "vv", "qmT", "mkT", "mv", "hdT", "g8", "outT")}
            S.barrier()
            for l in range(L):
                self.reset_region("A", PB, TOTAL)
                self.phase_A(l)
                S.barrier()
                self.reset_region("B", PB, TOTAL)
                self.phase_B(l)
                S.barrier()
                self.reset_region("C", PB, TOTAL)
                self.phase_C(l)
                S.barrier()
            S.emit()
        return nc

    def load_weight(self, dst, src_rows, ncols, kchunks, eng_cycle, B_w, stage_bufs, col0=0, pw=2048):
        S = self.S
        pieces = []
        for k in range(kchunks):
            c = 0
            while c < ncols:
                w = min(pw, ncols - c)
                pieces.append((k, c, w))
                c += w
        for i, (k, c, w) in enumerate(pieces):
            stg, B_stg, dsem = stage_bufs[i % len(stage_bufs)]
            S.dma("sp", dsem, self.dmaf(stg[:, 0:w], src_rows(k)[:, col0 + c:col0 + c + w]), (), (B_stg,))
            eng = eng_cycle[i % len(eng_cycle)]
            self.copy(eng, dst[:, k, c:c + w], stg[:, 0:w], (B_stg,), (B_w,))

    def rmsnorm(self, hch, B_h, gain_ap_fn, N, xn, B_xn, sq, B_sq, psb, rstd, B_rstd):
        ps_t = self.ps[psb]
        B_p = self.B_ps[psb]
        for c in range(NC8):
            self.act(sq[:, c, 0:N], hch[:, c, 0:N], AF.Square, (B_h,), (B_sq,))
        for c in range(NC8):
            self.mm(ps_t[:, 0:N], self.onesm[:], sq[:, c, 0:N], c == 0, c == NC8 - 1, (B_sq, self.B_const), (B_p,))
        self.act(rstd[:, 0:N], ps_t[:, 0:N], AF.Ln, (B_p, self.B_const), (B_rstd,), bias=self.epsb[:, 0:1], scale=1.0)
        self.act(rstd[:, 0:N], rstd[:, 0:N], AF.Exp, (B_rstd,), (B_rstd,), scale=-0.5)
        for c in range(NC8):
            g = gain_ap_fn(c)
            self.S.op("dve", (lambda o, i0, sc, i1: (lambda e: e.scalar_tensor_tensor(out=o, in0=i0, scalar=sc, in1=i1, op0=ALU.mult, op1=ALU.mult)))(
                xn[:, c, 0:N], hch[:, c, 0:N], g, rstd[:, 0:N]), (B_h, B_rstd, self.B_const), (B_xn,))

    def phase_A(self, l):
        S = self.S
        SL, NT, NQ = self.SL, self.NT, self.NQ
        fox = (l % 2 == 0)
        j = l // 2
        PW = FOXP if fox else MOBAP
        wsrc = self.w_fox if fox else self.w_moba
        QM0 = 3 * SW + (NH if fox else 0)
        R = "A"
        win = self.sb([128, 8, PW], BF16, R)
        wmk = self.sb([128, 8, 2 * MW], BF16, R)
        stg = [(self.sb([128, 2048], F32, R), Buf("stg%d" % i), self.dsem_w[i]) for i in range(2)]
        hch = [self.sb([128, 8, 512], F32, R) for _ in range(2)]
        B_hch = [Buf("hch0"), Buf("hch1")]
        sq = self.sb([128, 8, 512], BF16, R)
        B_sq = Buf("sq")
        xn2 = [self.sb([128, 8, 512], BF16, R) for _ in range(2)]
        B_xn2 = [Buf("xn0"), Buf("xn1")]
        xn = xn2[0]
        B_xn = B_xn2[0]
        rstd = self.sb([128, 512], F32, R)
        B_rstd = Buf("rstd")
        ev = [self.sb([128, 512], BF16, R) for _ in range(4)]
        B_ev = [Buf("ev%d" % i) for i in range(4)]
        vst = [self.sb([128, SW], BF16, R) for _ in range(2)]
        B_vst = [Buf("vst%d" % i) for i in range(2)]
        B_win = Buf("win")
        B_wmk = Buf("wmk")
        if fox:
            gT = self.sb([NH, SL], F32, R)
            B_gT = Buf("gT")
            g8t = self.sb([NH, SL], BF16, R)
            B_g8t = Buf("g8t")
            onesrow = self.sb([NH, 512], F32, R)
            B_onesrow = Buf("onesrow")
            self.memset("dve", onesrow[:], 1.0, (B_onesrow,))
        hsrc = self.xT if l == 0 else self.hT
        self.load_weight(wmk, lambda k: self.w_mkv[l, k * 128:(k + 1) * 128, :], 2 * MW, 8, ["pool", "dve"], B_wmk, stg)
        self.load_weight(win, lambda k: wsrc[j, k * 128:(k + 1) * 128, :], PW, 8, ["pool", "dve", "act"], B_win, stg)
        evc = [0]
        vsc = [0]

        def evac_store(ps_ap, rows, N, dram_ap, B_p, dkey, use):
            i = evc[0] % 4
            evc[0] += 1
            eng = "act" if use % 2 == 0 else "dve"
            self.copy(eng, ev[i][0:rows, 0:N], ps_ap, (B_p,), (B_ev[i],))
            S.dma("pool", self.dsem_st[i], self.dmaf(dram_ap, ev[i][0:rows, 0:N]), (B_ev[i],), (self.B_dram[dkey],))

        S.dma("sp", self.dsem_ld[0], self.dmaf(hch[0][:, :, 0:NMEM], self.memT.rearrange("(c p) n -> p c n", p=128)), (), (B_hch[0],))
        self.rmsnorm(hch[0], B_hch[0], lambda c: self.gmem[:, l * 8 + c:l * 8 + c + 1], NMEM, xn, B_xn, sq, B_sq, 7, rstd, B_rstd)
        pi = 0
        for ft in range(2):
            pb = pi % 6
            pi += 1
            for c in range(NC8):
                self.mm(self.ps[pb][:, 0:NMEM], wmk[:, c, ft * 128:(ft + 1) * 128], xn[:, c, 0:NMEM], c == 0, c == 7, (B_wmk, B_xn), (self.B_ps[pb],))
            evac_store(self.ps[pb][:, 0:NMEM], 128, NMEM, self.mkT[ft * 128:(ft + 1) * 128, :], self.B_ps[pb], "mkT", ft)
        for nt in range(2):
            pb = pi % 6
            pi += 1
            for c in range(NC8):
                self.mm(self.ps[pb][:, 0:MW], xn[:, c, nt * 128:(nt + 1) * 128], wmk[:, c, MW:2 * MW], c == 0, c == 7, (B_wmk, B_xn), (self.B_ps[pb],))
            evac_store(self.ps[pb][:, 0:MW], 128, MW, self.mv[nt * 128:(nt + 1) * 128, :], self.B_ps[pb], "mv", nt)

        def load_chunk(I):
            b = I % 2
            S.dma("sp", self.dsem_ld[b], self.dmaf(hch[b][:], hsrc[:, I * 512:(I + 1) * 512].rearrange("(c p) n -> p c n", p=128)),
                  (self.B_dram["hT"],), (B_hch[b],))
        load_chunk(0)
        if NQ > 1:
            load_chunk(1)

        def norm_chunk(I):
            self.rmsnorm(hch[I % 2], B_hch[I % 2], lambda c: self.gmix[:, l * 8 + c:l * 8 + c + 1], 512, xn2[I % 2], B_xn2[I % 2], sq, B_sq, 7, rstd, B_rstd)
        norm_chunk(0)
        for I in range(NQ):
            b = I % 2
            xn = xn2[b]
            B_xn = B_xn2[b]
            t0 = I * 512
            tiles = [("qT", self.qT, ft, ft * 128) for ft in range(6)] + [("kT", self.kT, ft, SW + ft * 128) for ft in range(6)] + \
                    [("qmT", self.qmT, ft, QM0 + ft * 128) for ft in range(2)]
            for ti, (dkey, dram, ft, col) in enumerate(tiles):
                if ti == 6 and I + 1 < NQ:
                    norm_chunk(I + 1)
                    if I + 2 < NQ:
                        load_chunk(I + 2)
                pb = pi % 6
                pi += 1
                for c in range(NC8):
                    self.mm(self.ps[pb][:, :], win[:, c, col:col + 128], xn[:, c, :], c == 0, c == 7, (B_win, B_xn), (self.B_ps[pb],))
                evac_store(self.ps[pb][:, :], 128, 512, dram[ft * 128:(ft + 1) * 128, t0:t0 + 512], self.B_ps[pb], dkey, pi)
            for tt_ in range(4):
                vi = vsc[0] % 2
                vsc[0] += 1
                for half, (c0, wd) in enumerate(((0, 512), (512, 256))):
                    pb = pi % 6
                    pi += 1
                    for c in range(NC8):
                        self.mm(self.ps[pb][:, 0:wd], xn[:, c, tt_ * 128:(tt_ + 1) * 128], win[:, c, 2 * SW + c0:2 * SW + c0 + wd], c == 0, c == 7,
                                (B_win, B_xn), (self.B_ps[pb],))
                    self.copy("act" if half == 0 else "dve", vst[vi][:, c0:c0 + wd], self.ps[pb][:, 0:wd], (self.B_ps[pb],), (B_vst[vi],))
                S.dma("pool", self.dsem_st[4 + vi], self.dmaf(self.vv[t0 + tt_ * 128:t0 + (tt_ + 1) * 128, :], vst[vi][:]), (B_vst[vi],), (self.B_dram["vv"],))
            if fox:
                pb = 6
                for c in range(NC8):
                    self.mm(self.ps[pb][0:NH, :], win[:, c, 3 * SW:3 * SW + NH], xn[:, c, :], c == 0, c == 7, (B_win, B_xn), (self.B_ps[pb],))
                self.act(gT[:, t0:t0 + 512], self.ps[pb][0:NH, :], AF.Exp, (self.B_ps[pb], self.B_const, B_gT), (B_gT,), bias=self.nbfg[:, j:j + 1], scale=-1.0)
                self.act(gT[:, t0:t0 + 512], gT[:, t0:t0 + 512], AF.Ln, (B_gT,), (B_gT,), bias=1.0, scale=1.0)
                init = 0.0 if I == 0 else gT[:, t0 - 1:t0]
                S.op("dve", (lambda o, d0, d1, ini: (lambda e: e.tensor_tensor_scan(out=o, data0=d0, data1=d1, initial=ini, op0=ALU.mult, op1=ALU.add)))(
                    gT[:, t0:t0 + 512], onesrow[:, :], gT[:, t0:t0 + 512], init), (B_gT, B_onesrow), (B_gT,))
        if fox:
            self.ts("dve", g8t[:], gT[:], -8.0, None, ALU.mult, None, (B_gT,), (B_g8t,))
            S.dma("pool", self.dsem_misc2, self.dmaf(self.g8[:, :], g8t[:]), (B_g8t,), (self.B_dram["g8"],))
            gview = self.gtab[:].rearrange("p (h j) -> p j h", h=NH)
            for j0 in range(0, NT, 32):
                nj = min(32, NT - j0)
                pb = (j0 // 32) % 2
                for jj in range(nj):
                    jt = j0 + jj
                    self.mm(self.ps[pb][:, jj * NH:(jj + 1) * NH], gT[:, jt * 128:(jt + 1) * 128], self.identf[0:NH, 0:NH], True, True,
                            (B_gT, self.B_const), (self.B_ps[pb],))
                self.copy("dve", gview[:, j0:j0 + nj, :], self.ps[pb][:, 0:nj * NH].rearrange("p (j h) -> p j h", h=NH), (self.B_ps[pb],), (self.B_gtab,))

    def phase_B(self, l):
        S = self.S
        SL, NT, NQ, NB = self.SL, self.NT, self.NQ, self.NB
        fox = (l % 2 == 0)
        R = "B"
        KA = 97
        LA = 2
        KT = [self.sb([128, SL], BF16, R) for _ in range(2)]
        QT = [self.sb([128, SL], BF16, R) for _ in range(2)]
        VA = [self.sb([128, NT, 72], BF16, R) for _ in range(2)]
        B_K = [Buf("K0"), Buf("K1")]
        B_Q = [Buf("Q0"), Buf("Q1")]
        B_V = [Buf("V0"), Buf("V1")]
        B_Qm = [[Buf("Qm%d_%d" % (s, I)) for I in range(NQ)] for s in range(2)]
        PT = [self.sb([128, 512], BF16, R) for _ in range(4)]
        B_PT = [Buf("PT%d" % i) for i in range(4)]
        rrow = [self.sb([128, 512], F32, R) for _ in range(2)]
        B_rrow = [Buf("rrow0"), Buf("rrow1")]
        sbB = [self.sb([64, 512], F32, R) for _ in range(2)]
        B_sbB = [Buf("sbB0"), Buf("sbB1")]
        hst = [self.sb([64, 512], BF16, R) for _ in range(2)]
        B_hst = [Buf("hst0"), Buf("hst1")]
        zb = self.sb([128, 1], F32, R)
        B_zb = Buf("zb")
        self.memset("dve", zb[:], 0.0, (B_zb,))
        if not fox:
            gm = self.sb([128, 4, 32], F32, R)
            B_gm = Buf("gm")
            m8 = self.sb([128, 4, 8], F32, R)
            B_m8 = Buf("m8")
            thr = self.sb([128, 4], F32, R)
            B_thr = Buf("thr")
            stage = [self.sb([128, 96], BF16, R) for _ in range(4)]
            B_stage = [Buf("stage%d" % i) for i in range(4)]
            kms = self.sb([64, NB], F32, R)
            kmh = self.sb([64, NB], BF16, R)
            kml = self.sb([64, NB], BF16, R)
            kmhf = self.sb([64, NB], F32, R)
            B_km = Buf("km")
            akbh = [self.sb([128, NT], F32, R) for _ in range(2)]
            B_akbh = [Buf("akbh0"), Buf("akbh1")]
            for i in range(4):
                self.memset("dve", stage[i][:], 0.0, (B_stage[i],))
        for s in range(2):
            self.memset("dve", VA[s][:, :, 64:65], 1.0, (B_V[s],))
            self.memset("pool", KT[s][96:97, :], 1.0, (B_K[s],))
            if fox:
                self.memset("pool", QT[s][64:96, :], 0.0, (B_Q[s],))
        B_Kst = Buf("kstatic")
        S.dma_group("sp", self.dsem_misc, [(self.dmaf(KT[s_][64:96, :], self.c_onehot[:, :]), (), (B_Kst,)) for s_ in range(2)])
        psS = [0, 1, 2]
        psO = [3, 4]
        psBk = 5
        psG = 6
        psTr = 7
        heads = [("self", h) for h in range(NH)] + [("mem", h) for h in range(NMH)]

        def load_head(idx):
            kind, h = heads[idx]
            s = idx % 2
            items = []
            if kind == "self":
                items.append((self.dmaf(KT[s][0:64, :], self.kT[h * 64:(h + 1) * 64, :]), (self.B_dram["kT"],), (B_K[s],)))
                items.append((self.dmaf(QT[s][0:64, :], self.qT[h * 64:(h + 1) * 64, :]), (self.B_dram["qT"],), (B_Q[s],)))
                shift = self.g8[h:h + 1, :] if fox else self.c_aqs[h:h + 1, :]
                items.append((self.dmaf(QT[s][96:97, :], shift), (self.B_dram["g8"],), (B_Q[s],)))
                items.append((self.dmaf(VA[s][:, :, 0:64], self.vv[:, h * 64:(h + 1) * 64].rearrange("(j p) d -> p j d", p=128)),
                              (self.B_dram["vv"],), (B_V[s],)))
            else:
                items.append((self.dmaf(KT[s][0:64, 0:NMEM], self.mkT[h * 64:(h + 1) * 64, :]), (self.B_dram["mkT"],), (B_K[s],)))
                items.append((self.dmaf(QT[s][0:64, :], self.qmT[h * 64:(h + 1) * 64, :]), (self.B_dram["qmT"],), (B_Q[s],)))
                items.append((self.dmaf(VA[s][:, 0:2, 0:64], self.mv[:, h * 64:(h + 1) * 64].rearrange("(j p) d -> p j d", p=128)),
                              (self.B_dram["mv"],), (B_V[s],)))
            S.dma_group("sp", self.dsem_ld[s], items)

        def kmean(idx):
            s = idx % 2
            h = heads[idx][1]
            S.op("dve", (lambda o, i: (lambda e: e.tensor_reduce(out=o, in_=i, axis=AX.X, op=ALU.add)))(
                kms[:, :], KT[s][0:64, :].rearrange("d (n s) -> d n s", s=256)), (B_K[s],), (B_km,))
            self.ts("dve", kmh[:, :], kms[:, :], 1.0 / 256.0, None, ALU.mult, None, (B_km,), (B_km,))
            self.copy("dve", kmhf[:, :], kmh[:, :], (B_km,), (B_km,))
            S.op("dve", (lambda o, i0, i1: (lambda e: e.scalar_tensor_tensor(out=o, in0=i0, scalar=1.0 / 256.0, in1=i1, op0=ALU.mult, op1=ALU.subtract)))(
                kml[:, :], kms[:, :], kmhf[:, :]), (B_km,), (B_km,))
            self.ts("dve", akbh[s][:, :], self.akb[:, h * NT:(h + 1) * NT], 1.0, None, ALU.mult, None, (self.B_const,), (B_akbh[s],))

        def gate1(idx, I):
            s = idx % 2
            pg = self.ps[psG]
            B_pg = self.B_ps[psG]
            for qi in range(4):
                qt = 4 * I + qi
                self.mm(pg[:, qi * 32:qi * 32 + NB], QT[s][0:64, qt * 128:(qt + 1) * 128], kmh[:, :], True, False, (B_Q[s], B_km), (B_pg,))
                self.mm(pg[:, qi * 32:qi * 32 + NB], QT[s][0:64, qt * 128:(qt + 1) * 128], kml[:, :], False, True, (B_Q[s], B_km), (B_pg,))
            for qi in range(4):
                own = (4 * I + qi) // 2
                self.tt("dve", gm[:, qi, 0:NB], pg[:, qi * 32:qi * 32 + NB], self.bm[:, own * 32:own * 32 + NB], ALU.add, (B_pg, self.B_const), (B_gm,))
            for qi in range(4):
                S.op("dve", (lambda o, i: (lambda e: e.max(out=o, in_=i)))(m8[:, qi, :], gm[:, qi, 0:NB]), (B_gm,), (B_m8,))
            self.ts("dve", thr[:, :], m8[:, :, 3], -1e29, None, ALU.max, None, (B_m8,), (B_thr,))
            for qi in range(4):
                self.ts("dve", stage[qi][:, 64:64 + NB], gm[:, qi, 0:NB], thr[:, qi:qi + 1], MASKV, ALU.is_lt, ALU.mult, (B_gm, B_thr), (B_stage[qi],))

        def gate2(idx, I):
            s = idx % 2
            ptr = self.ps[psTr]
            B_ptr = self.B_ps[psTr]
            for qi in range(4):
                self.mm(ptr[0:96, qi * 128:(qi + 1) * 128], stage[qi][:, :], self.identb[:, :], True, True, (B_stage[qi], self.B_const), (B_ptr,))
            self.copy("act", QT[s][64:96, I * 512:(I + 1) * 512], ptr[64:96, :], (B_ptr,), (B_Qm[s][I],))

        units = []
        for idx, (kind, h) in enumerate(heads):
            k = 0
            for I in range(NQ):
                jl = list(range(4 * I + 4)) if kind == "self" else [0, 1]
                for ji, jt in enumerate(jl):
                    units.append((idx, I, ji, jt, len(jl), k))
                    k += 1
        n = len(units)
        state = {"step": 0}
        deferred = []

        def defer(k, fn, key=None):
            deferred.append((state["step"] + k, fn, key))

        def run_due(force=False, key=None):
            for ent in deferred[:]:
                due, fn, kk = ent
                if force or due <= state["step"] or (key is not None and kk == key):
                    deferred.remove(ent)
                    fn()

        def stage12(u, uid):
            idx, I, ji, jt, nj, k = u
            kind, h = heads[idx]
            s = idx % 2
            selfh = (kind == "self")
            moba = selfh and not fox
            K = KA if selfh else 64
            t0 = I * 512
            c0 = 0
            diag = False
            if selfh and jt >= 4 * I:
                c0 = 128 * (jt - 4 * I)
                diag = True
            sb_ = psS[uid % 3]
            pi_ = uid % 4
            pS = self.ps[sb_]
            rd = [B_K[s], B_Q[s], B_Kst]
            if moba:
                rd.append(B_Qm[s][I])
            self.mm(pS[:, c0:512], KT[s][0:K, jt * 128:(jt + 1) * 128], QT[s][0:K, t0 + c0:t0 + 512], True, not diag, rd, (self.B_ps[sb_],))
            if diag:
                self.mm(pS[:, c0:c0 + 128], self.identb[:, :], self.tri[:, :], False, True, (self.B_const,), (self.B_ps[sb_],))
            if not selfh:
                bias = zb[:, 0:1]
                rb = B_zb
            elif fox:
                bias = self.gtab[:, h * NT + jt:h * NT + jt + 1]
                rb = self.B_gtab
            else:
                bias = akbh[s][:, jt:jt + 1]
                rb = B_akbh[s]
            self.act(PT[pi_][:, c0:512], pS[:, c0:512], AF.Exp, (self.B_ps[sb_], rb), (B_PT[pi_],), bias=bias, scale=0.125)

        chunk_seq = {}

        def stage3(u, uid):
            idx, I, ji, jt, nj, k = u
            kind, h = heads[idx]
            s = idx % 2
            selfh = (kind == "self")
            t0 = I * 512
            c0 = 0
            if selfh and jt >= 4 * I:
                c0 = 128 * (jt - 4 * I)
            cs = idx * NQ + I
            o = psO[cs % 2]
            po = self.ps[o]
            B_po = self.B_ps[o]
            pi_ = uid % 4
            if ji == 0:
                run_due(key=("fin", o))
            self.mm(po[0:65, c0:512], VA[s][:, jt, 0:65], PT[pi_][:, c0:512], ji == 0, ji == nj - 1, (B_V[s], B_PT[pi_]), (B_po,))
            if k == 0 and idx + 1 < len(heads):
                load_head(idx + 1)
            if ji == nj - 1:
                hi = cs % 2
                S.op("dve", (lambda o_, i_: (lambda e: e.reciprocal(out=o_, in_=i_)))(rrow[hi][64:65, :], po[64:65, :]), (B_po,), (B_rrow[hi],))

                def fin(hi=hi, po=po, B_po=B_po, h=h, selfh=selfh, t0=t0):
                    pb_ = self.ps[psBk]
                    self.mm(pb_[0:64, :], self.onesf[64:65, 0:64], rrow[hi][64:65, :], True, True, (B_rrow[hi], self.B_const), (self.B_ps[psBk],))
                    self.copy("act", sbB[hi][:, :], pb_[0:64, :], (self.B_ps[psBk],), (B_sbB[hi],))
                    self.tt("dve", hst[hi][:, :], po[0:64, :], sbB[hi][:, :], ALU.mult, (B_po, B_sbB[hi]), (B_hst[hi],))
                    row0 = h * 64 if selfh else SW + h * 64
                    S.dma("pool", self.dsem_st[hi], self.dmaf(self.hdT[row0:row0 + 64, t0:t0 + 512], hst[hi][:, :]), (B_hst[hi],), (self.B_dram["hdT"],))
                defer(4, fin, key=("fin", o))

        nhu = sum(4 * I + 4 for I in range(NQ))
        k_km = max(LA + 1, min(64, nhu // 8))
        k_g0 = k_km + max(1, min(16, nhu // 16))
        g_stride = max(1, min(24, (nhu - k_g0 - 12) // NQ))
        load_head(0)
        if not fox:
            kmean(0)
            for I in range(NQ):
                gate1(0, I)
                gate2(0, I)
        for step in range(n + LA):
            state["step"] = step
            if step < n:
                u = units[step]
                idx, I, ji, jt, nj, k = u
                if (not fox) and idx + 1 < NH:
                    if k == k_km:
                        kmean(idx + 1)
                    g, r = divmod(k - k_g0, g_stride)
                    if k >= k_g0 and r == 0 and g < NQ:
                        run_due(key="gate2")
                        gate1(idx + 1, g)
                        defer(10, (lambda a, b: (lambda: gate2(a, b)))(idx + 1, g), key="gate2")
                stage12(u, step)
            if step - LA >= 0:
                stage3(units[step - LA], step - LA)
            run_due()
        run_due(force=True)

    def phase_C(self, l):
        S = self.S
        SL = self.SL
        R = "C"
        N = 256
        NCH = SL // N
        last = (l == self.depth - 1)
        wo = self.sb([128, 8, D], BF16, R)
        wgu = self.sb([128, 8, 2 * DFF], BF16, R)
        wdn = self.sb([128, 22, D], BF16, R)
        B_wo, B_wgu, B_wdn = Buf("wo"), Buf("wgu"), Buf("wdn")
        stg = [(self.sb([128, 1024], F32, R), Buf("stgc%d" % i), self.dsem_w[i]) for i in range(2)]
        hch = [self.sb([128, 8, N], F32, R) for _ in range(2)]
        B_hch = [Buf("hc0"), Buf("hc1")]
        hdc1 = self.sb([128, 8, N], BF16, R)
        hdc = [hdc1, hdc1]
        B_hd1 = Buf("hd")
        B_hdc = [B_hd1, B_hd1]
        hn = self.sb([128, 8, N], BF16, R)
        B_hn = Buf("hn")
        sq = hn
        B_sq = B_hn
        rstd = self.sb([128, N], F32, R)
        B_rstd = Buf("rstdc")
        aT = self.sb([128, 22, N], BF16, R)
        B_aT = Buf("aT")
        gact = [self.sb([128, N], F32, R) for _ in range(2)]
        B_gact = [Buf("ga%d" % i) for i in range(2)]
        hsrc = self.xT if l == 0 else self.hT
        self.load_weight(wo, lambda k: self.w_out[l, k * 128:(k + 1) * 128, :], D, 8, ["pool", "dve", "act"], B_wo, stg, pw=1024)
        self.load_weight(wgu, lambda k: self.w_gu[l, k * 128:(k + 1) * 128, :], 2 * DFF, 8, ["pool", "dve", "act"], B_wgu, stg, pw=1024)
        self.load_weight(wdn, lambda k: self.w_dn[l, k * 128:(k + 1) * 128, :], D, 22, ["pool", "dve", "act"], B_wdn, stg, pw=1024)

        def load_chunk(I):
            b = I % 2
            S.dma("sp", self.dsem_ld[b], self.dmaf(hch[b][:], hsrc[:, I * N:(I + 1) * N].rearrange("(c p) n -> p c n", p=128)),
                  (self.B_dram["hT"],), (B_hch[b],))

        def load_hd(I):
            b = I % 2
            S.dma("sp", self.dsem_ld[2 + b], self.dmaf(hdc[b][:], self.hdT[:, I * N:(I + 1) * N].rearrange("(c p) n -> p c n", p=128)),
                  (self.B_dram["hdT"],), (B_hdc[b],))
        load_chunk(0)
        load_hd(0)
        if NCH > 1:
            load_chunk(1)
        st_ = {"pi": 0, "ga": 0}

        def outproj(I):
            b = I % 2
            for fo in range(8):
                pb = st_["pi"] % 7
                st_["pi"] += 1
                for c in range(8):
                    self.mm(self.ps[pb][:, 0:N], wo[:, c, fo * 128:(fo + 1) * 128], hdc[b][:, c, :], c == 0, c == 7, (B_wo, B_hdc[b]), (self.B_ps[pb],))
                self.tt("dve", hch[b][:, fo, :], self.ps[pb][:, 0:N], hch[b][:, fo, :], ALU.add, (self.B_ps[pb], B_hch[b]), (B_hch[b],))
            if I + 1 < NCH:
                load_hd(I + 1)

        def norm(I):
            b = I % 2
            self.rmsnorm(hch[b], B_hch[b], lambda c: self.gffn[:, l * 8 + c:l * 8 + c + 1], N, hn, B_hn, sq, B_sq, 7, rstd, B_rstd)

        def down(I, fos):
            b = I % 2
            for fo in fos:
                pb = st_["pi"] % 7
                st_["pi"] += 1
                for k in range(22):
                    self.mm(self.ps[pb][:, 0:N], wdn[:, k, fo * 128:(fo + 1) * 128], aT[:, k, :], k == 0, k == 21, (B_wdn, B_aT), (self.B_ps[pb],))
                self.tt("dve", hch[b][:, fo, :], self.ps[pb][:, 0:N], hch[b][:, fo, :], ALU.add, (self.B_ps[pb], B_hch[b]), (B_hch[b],))

        outproj(0)
        norm(0)
        for I in range(NCH):
            b = I % 2
            t0 = I * N
            for ft in range(22):
                pg = st_["pi"] % 7
                st_["pi"] += 1
                for c in range(8):
                    self.mm(self.ps[pg][:, 0:N], wgu[:, c, ft * 128:(ft + 1) * 128], hn[:, c, :], c == 0, c == 7, (B_wgu, B_hn), (self.B_ps[pg],))
                gi = st_["ga"] % 2
                st_["ga"] += 1
                self.act(gact[gi][:, :], self.ps[pg][:, 0:N], AF.Silu, (self.B_ps[pg],), (B_gact[gi],))
                pu = st_["pi"] % 7
                st_["pi"] += 1
                for c in range(8):
                    self.mm(self.ps[pu][:, 0:N], wgu[:, c, DFF + ft * 128:DFF + (ft + 1) * 128], hn[:, c, :], c == 0, c == 7, (B_wgu, B_hn), (self.B_ps[pu],))
                self.tt("dve", aT[:, ft, :], self.ps[pu][:, 0:N], gact[gi][:, :], ALU.mult, (self.B_ps[pu], B_gact[gi]), (B_aT,))
            if I + 1 < NCH:
                outproj(I + 1)
            down(I, range(0, 4))
            if I + 1 < NCH:
                norm(I + 1)
            down(I, range(4, 8))
            if not last:
                S.dma("pool", self.dsem_st[b], self.dmaf(self.hT[:, t0:t0 + N].rearrange("(c p) n -> p c n", p=128), hch[b][:]),
                      (B_hch[b],), (self.B_dram["hT"],))
            else:
                self.final_norm(hch[b], B_hch[b], N, aT, B_aT, rstd, B_rstd)
                S.dma("pool", self.dsem_st[b], self.dmaf(self.outT[:, t0:t0 + N].rearrange("(c p) n -> p c n", p=128), hch[b][:]),
                      (B_hch[b],), (self.B_dram["outT"],))
            if I + 2 < NCH:
                load_chunk(I + 2)

    def final_norm(self, hch, B_h, N, sq, B_sq, rstd, B_rstd):
        ps_t = self.ps[7]
        B_p = self.B_ps[7]
        for c in range(NC8):
            self.act(sq[:, c, 0:N], hch[:, c, 0:N], AF.Square, (B_h,), (B_sq,))
        for c in range(NC8):
            self.mm(ps_t[:, 0:N], self.onesm[:], sq[:, c, 0:N], c == 0, c == NC8 - 1, (B_sq, self.B_const), (B_p,))
        self.act(rstd[:, 0:N], ps_t[:, 0:N], AF.Ln, (B_p, self.B_const), (B_rstd,), bias=self.epsb[:, 0:1], scale=1.0)
        self.act(rstd[:, 0:N], rstd[:, 0:N], AF.Exp, (B_rstd,), (B_rstd,), scale=-0.5)
        for c in range(NC8):
            self.S.op("dve", (lambda o, i0, sc, i1: (lambda e: e.scalar_tensor_tensor(out=o, in0=i0, scalar=sc, in1=i1, op0=ALU.mult, op1=ALU.mult)))(
                hch[:, c, 0:N], hch[:, c, 0:N], self.gfin[:, c:c + 1], rstd[:, 0:N]), (B_h, B_rstd, self.B_const), (B_h,))


def _consts(SL):
    NT = SL // 128
    NB = SL // 256
    bf = ml_dtypes.bfloat16
    c = {}
    c["c_identb"] = np.eye(128, dtype=np.float32).astype(bf)
    c["c_identf"] = np.eye(128, dtype=np.float32)
    s = np.arange(128)
    c["c_tri"] = np.where(s[None, :] >= s[:, None], 0.0, MASKV).astype(np.float32).astype(bf)
    pos = np.arange(SL)
    c["c_onehot"] = (pos[None, :] // 256 == np.arange(32)[:, None]).astype(np.float32).astype(bf)
    slopes = (2.0 ** (-8.0 * np.arange(1, NH + 1, dtype=np.float32) / NH)).astype(np.float32)
    p = np.arange(128, dtype=np.float32)
    jj = np.arange(NT, dtype=np.float32)
    kb = slopes[None, :, None] * (128.0 * jj[None, None, :] + p[:, None, None])
    c["c_alibi_kbias"] = kb.reshape(128, NH * NT).astype(np.float32)
    c["c_alibi_qshift"] = (-8.0 * slopes[:, None] * pos[None, :].astype(np.float32)).astype(np.float32).astype(bf)
    bm = np.zeros((NB, 32), np.float32)
    for own in range(NB):
        bm[own, own] = 1e30
        bm[own, own + 1:] = -1e30
    c["c_moba_biasmask"] = np.ascontiguousarray(np.broadcast_to(bm.reshape(1, NB * 32), (128, NB * 32))).astype(np.float32)
    return c


def _gain_layout(g):
    L = g.shape[0]
    return np.ascontiguousarray(g.reshape(L, 8, 128).transpose(2, 0, 1).reshape(128, L * 8)).astype(np.float32)


_CACHE = {}


def run(x, mem, norm_mix, norm_mem, norm_ffn, norm_final, w_in_fox, b_fgate, w_in_moba, w_mem_kv, w_out,
        w_gate_up, w_down, n_cores=8):
    B, SL, _ = x.shape
    depth = norm_mix.shape[0]
    key = (SL, depth)
    if key not in _CACHE:
        _CACHE[key] = Prog(SL, depth).build()
    nc = _CACHE[key]
    consts = _consts(SL)
    shared = dict(consts)
    shared["g_mix"] = _gain_layout(np.asarray(norm_mix))
    shared["g_mem"] = _gain_layout(np.asarray(norm_mem))
    shared["g_ffn"] = _gain_layout(np.asarray(norm_ffn))
    shared["g_fin"] = _gain_layout(np.asarray(norm_final)[None, :])
    shared["w_in_fox"] = np.ascontiguousarray(w_in_fox, dtype=np.float32)
    shared["b_fgate"] = np.ascontiguousarray(np.asarray(b_fgate).T, dtype=np.float32)
    wm = np.asarray(w_in_moba, dtype=np.float32)
    if wm.shape[0] == 0:
        wm = np.zeros((1, D, MOBAP), np.float32)
    shared["w_in_moba"] = np.ascontiguousarray(wm)
    shared["w_mem_kv"] = np.ascontiguousarray(w_mem_kv, dtype=np.float32)
    shared["w_out"] = np.ascontiguousarray(w_out, dtype=np.float32)
    shared["w_gate_up"] = np.ascontiguousarray(w_gate_up, dtype=np.float32)
    shared["w_down"] = np.ascontiguousarray(w_down, dtype=np.float32)
    work = [0, 1, 4, 5][:B] if n_cores == 8 else list(range(B))
    idle = None
    in_maps = []
    for cidx in range(n_cores):
        if cidx in work:
            b = work.index(cidx)
            m = dict(shared)
            m["xT"] = np.ascontiguousarray(np.asarray(x[b]).T, dtype=np.float32)
            m["memT"] = np.ascontiguousarray(np.asarray(mem[b]).T, dtype=np.float32)
        else:
            if idle is None:
                idle = {k: (v if k.startswith("c_") else np.zeros_like(v)) for k, v in shared.items()}
                idle["xT"] = np.zeros((D, SL), np.float32)
                idle["memT"] = np.zeros((D, NMEM), np.float32)
            m = idle
        in_maps.append(m)
    res = run_bass_kernel_spmd(nc, in_maps, core_ids=list(range(n_cores)))
    out = np.empty((B, SL, D), np.float32)
    for b in range(B):
        out[b] = res.results[work[b]]["outT"].T
    return out


def kernel(x, mem, norm_mix, norm_mem, norm_ffn, norm_final, w_in_fox, b_fgate, w_in_moba, w_mem_kv, w_out,
           w_gate_up, w_down):
    return run(x, mem, norm_mix, norm_mem, norm_ffn, norm_final, w_in_fox, b_fgate, w_in_moba, w_mem_kv, w_out,
               w_gate_up, w_down, n_cores=8)
```

```python
import numpy as np
import ml_dtypes
from contextlib import ExitStack
import concourse.bass as bass
import concourse.mybir as mybir
from concourse.bass_utils import run_bass_kernel_spmd

F32 = mybir.dt.float32
BF16 = mybir.dt.bfloat16
ALU = mybir.AluOpType
AF = mybir.ActivationFunctionType
AX = mybir.AxisListType

D = 1024
NH = 12
NMH = 4
HD = 64
SW = 768
MW = 256
NMEM = 256
DFF = 2816
FOXP = 2572
MOBAP = 2560
EPS = 1e-6
MASKV = -29952.0
NC8 = 8

COMPUTE = ("pe", "act", "dve", "pool")


class Buf:
    __slots__ = ("name", "lw", "rd")

    def __init__(self, name=""):
        self.name = name
        self.lw = None
        self.rd = []


class DmaSem:
    __slots__ = ("sem", "n")

    def __init__(self, sem):
        self.sem = sem
        self.n = 0


class Sched:
    def __init__(self, nc, stack):
        self.nc = nc
        self.stack = stack
        self.q = {e: [] for e in ("pe", "act", "dve", "pool", "sp")}
        self.psem = {}
        self.cnt = {}
        for e in COMPUTE:
            self.psem[e] = stack.enter_context(nc.semaphore("prog_" + e))
            self.cnt[e] = 0
        self.waited = {e: {} for e in self.q}
        self.dsems = []
        self.ninstr = 0

    def dmasem(self, name):
        s = self.stack.enter_context(self.nc.semaphore("d_" + name))
        d = DmaSem(s)
        self.dsems.append(d)
        return d

    def _deps(self, eng, reads, writes):
        deps = []
        for r in reads:
            if r.lw is not None:
                deps.append(r.lw)
        for w in writes:
            if w.lw is not None:
                deps.append(w.lw)
            deps.extend(w.rd)
        need = {}
        for (sem, val, teng) in deps:
            if eng == "pe" and teng == "pe":
                continue
            k = id(sem)
            if k not in need or need[k][1] < val:
                need[k] = (sem, val)
        waits = []
        wd = self.waited[eng]
        for k, (sem, val) in need.items():
            if wd.get(k, 0) >= val:
                continue
            wd[k] = val
            waits.append((sem, val))
        return waits

    @staticmethod
    def _commit(tok, reads, writes):
        for r in reads:
            r.rd.append(tok)
        for w in writes:
            w.lw = tok
            w.rd = []

    def op(self, eng, fn, reads=(), writes=()):
        waits = self._deps(eng, reads, writes)
        self.cnt[eng] += 1
        tok = (self.psem[eng], self.cnt[eng], eng)
        self.q[eng].append((waits, fn, (self.psem[eng], 1)))
        self._commit(tok, reads, writes)
        self.ninstr += 1
        return tok

    def dma_group(self, q, dsem, items):
        for fn, reads, writes in items:
            waits = self._deps(q, reads, writes)
            dsem.n += 1
            self.q[q].append((waits, fn, (dsem.sem, 16)))
            self.ninstr += 1
        tok = (dsem.sem, 16 * dsem.n, "dma")
        for fn, reads, writes in items:
            self._commit(tok, reads, writes)
        return tok

    def dma(self, q, dsem, fn, reads=(), writes=()):
        return self.dma_group(q, dsem, [(fn, reads, writes)])

    def barrier(self):
        for e in self.q:
            waits = []
            wd = self.waited[e]
            for o in COMPUTE:
                if o == e or self.cnt[o] == 0:
                    continue
                k = id(self.psem[o])
                if wd.get(k, 0) < self.cnt[o]:
                    wd[k] = self.cnt[o]
                    waits.append((self.psem[o], self.cnt[o]))
            for d in self.dsems:
                if d.n == 0:
                    continue
                k = id(d.sem)
                if wd.get(k, 0) < 16 * d.n:
                    wd[k] = 16 * d.n
                    waits.append((d.sem, 16 * d.n))
            if waits:
                self.q[e].append((waits, None, None))

    def emit(self):
        nc = self.nc
        q = self.q

        def run(engobj, lst):
            for waits, fn, inc in lst:
                for sem, val in waits:
                    engobj.wait_ge(sem, val)
                if fn is not None:
                    ins = fn(engobj)
                    ins.then_inc(inc[0], inc[1])

        with nc.Block() as block:
            @block.sync
            def _(e):
                run(e, q["sp"])

            @block.tensor
            def _(e):
                run(e, q["pe"])

            @block.scalar
            def _(e):
                run(e, q["act"])

            @block.vector
            def _(e):
                run(e, q["dve"])

            @block.gpsimd
            def _(e):
                run(e, q["pool"])


class Prog:
    def __init__(self, S_len, depth):
        self.SL = S_len
        self.depth = depth
        self.NT = S_len // 128
        self.NQ = S_len // 512
        self.NB = S_len // 256

    def mm(self, out, lhsT, rhs, start, stop, reads, writes):
        return self.S.op("pe", lambda e: e.matmul(out, lhsT=lhsT, rhs=rhs, start=start, stop=stop), reads, writes)

    def act(self, out, in_, func, reads, writes, bias=None, scale=None, eng="act"):
        kw = {}
        if bias is not None:
            kw["bias"] = bias
        if scale is not None:
            kw["scale"] = scale
        return self.S.op("act", lambda e: e.activation(out=out, in_=in_, func=func, **kw), reads, writes)

    def copy(self, eng, out, in_, reads, writes):
        if eng == "act":
            return self.S.op("act", lambda e: e.copy(out=out, in_=in_), reads, writes)
        return self.S.op(eng, lambda e: e.tensor_copy(out=out, in_=in_), reads, writes)

    def tt(self, eng, out, in0, in1, op, reads, writes):
        return self.S.op(eng, lambda e: e.tensor_tensor(out=out, in0=in0, in1=in1, op=op), reads, writes)

    def ts(self, eng, out, in0, s1, s2, op0, op1, reads, writes):
        if op1 is None:
            return self.S.op(eng, lambda e: e.tensor_scalar(out=out, in0=in0, scalar1=s1, scalar2=None, op0=op0), reads, writes)
        return self.S.op(eng, lambda e: e.tensor_scalar(out=out, in0=in0, scalar1=s1, scalar2=s2, op0=op0, op1=op1), reads, writes)

    def memset(self, eng, ap, val, writes):
        return self.S.op(eng, lambda e: e.memset(ap, val), (), writes)

    def dmaf(self, out, in_):
        return lambda e: e.dma_start(out=out, in_=in_)

    def sb(self, shape, dt, region):
        nbytes = int(np.prod(shape[1:])) * (4 if dt == F32 else 2)
        nbytes = (nbytes + 63) // 64 * 64
        off = self.off[region]
        self.off[region] = off + nbytes
        assert self.off[region] <= self.lim[region], (region, self.off[region], self.lim[region])
        self.nt += 1
        return self.nc.alloc_sbuf_tensor_at("t%d" % self.nt, list(shape), dt, offset=off)

    def reset_region(self, region, base, lim):
        self.off[region] = base
        self.lim[region] = lim

    def build(self):
        SL, NT, NQ, NB = self.SL, self.NT, self.NQ, self.NB
        nc = bass.Bass("TRN2", target_bir_lowering=False)
        self.nc = nc
        dt_in = lambda name, shape, dt=F32: nc.dram_tensor(name, list(shape), dt, kind="ExternalInput").ap()
        dscr = lambda name, shape, dt: nc.dram_tensor(name, list(shape), dt, kind="Internal").ap()
        L = self.depth
        NF = (L + 1) // 2
        NM = L // 2
        self.xT = dt_in("xT", [D, SL])
        self.memT = dt_in("memT", [D, NMEM])
        self.g_mix = dt_in("g_mix", [128, L * 8])
        self.g_mem = dt_in("g_mem", [128, L * 8])
        self.g_ffn = dt_in("g_ffn", [128, L * 8])
        self.g_fin = dt_in("g_fin", [128, 8])
        self.w_fox = dt_in("w_in_fox", [NF, D, FOXP])
        self.bfg = dt_in("b_fgate", [NH, NF])
        self.w_moba = dt_in("w_in_moba", [max(NM, 1), D, MOBAP])
        self.w_mkv = dt_in("w_mem_kv", [L, D, 2 * MW])
        self.w_out = dt_in("w_out", [L, D, D])
        self.w_gu = dt_in("w_gate_up", [L, D, 2 * DFF])
        self.w_dn = dt_in("w_down", [L, DFF, D])
        self.c_identb = dt_in("c_identb", [128, 128], BF16)
        self.c_identf = dt_in("c_identf", [128, 128])
        self.c_tri = dt_in("c_tri", [128, 128], BF16)
        self.c_onehot = dt_in("c_onehot", [32, SL], BF16)
        self.c_akb = dt_in("c_alibi_kbias", [128, NH * NT])
        self.c_aqs = dt_in("c_alibi_qshift", [NH, SL], BF16)
        self.c_bm = dt_in("c_moba_biasmask", [128, NB * 32])
        self.outT = nc.dram_tensor("outT", [D, SL], F32, kind="ExternalOutput").ap()
        self.hT = dscr("hT", [D, SL], F32)
        self.qT = dscr("qT", [SW, SL], BF16)
        self.kT = dscr("kT", [SW, SL], BF16)
        self.vv = dscr("vv", [SL, SW], BF16)
        self.qmT = dscr("qmT", [MW, SL], BF16)
        self.mkT = dscr("mkT", [MW, NMEM], BF16)
        self.mv = dscr("mv", [NMEM, MW], BF16)
        self.hdT = dscr("hdT", [D, SL], BF16)
        self.g8 = dscr("g8", [NH, SL], BF16)

        with ExitStack() as st:
            S = Sched(nc, st)
            self.S = S
            self.nt = 0
            self.off = {}
            self.lim = {}
            TOTAL = 229376
            self.reset_region("P", 16640, 16640 + 12800)
            PB = 16640 + 12800
            P = self
            self.identb = self.sb([128, 128], BF16, "P")
            self.identf = self.sb([128, 128], F32, "P")
            self.tri = self.sb([128, 128], BF16, "P")
            self.onesm = self.sb([128, 128], BF16, "P")
            self.onesf = self.sb([128, 64], F32, "P")
            self.gmix = self.sb([128, L * 8], F32, "P")
            self.gmem = self.sb([128, L * 8], F32, "P")
            self.gffn = self.sb([128, L * 8], F32, "P")
            self.gfin = self.sb([128, 8], F32, "P")
            self.nbfg = self.sb([NH, NF], F32, "P")
            self.akb = self.sb([128, NH * NT], F32, "P")
            self.bm = self.sb([128, NB * 32], F32, "P")
            self.gtab = self.sb([128, NH * NT], F32, "P")
            self.epsb = self.sb([128, 1], F32, "P")
            self.B_const = Buf("const")
            self.B_gtab = Buf("gtab")
            csem = S.dmasem("const")
            items = []
            for dst, src in ((self.identb, self.c_identb), (self.identf, self.c_identf), (self.tri, self.c_tri),
                             (self.gmix, self.g_mix), (self.gmem, self.g_mem), (self.gffn, self.g_ffn),
                             (self.gfin, self.g_fin), (self.nbfg, self.bfg), (self.akb, self.c_akb),
                             (self.bm, self.c_bm)):
                items.append((self.dmaf(dst[:], src), (), (self.B_const,)))
            S.dma_group("sp", csem, items)
            self.memset("dve", self.onesm[:], 1.0 / 1024.0, (self.B_const,))
            self.memset("dve", self.onesf[:], 1.0, (self.B_const,))
            self.memset("dve", self.epsb[:], EPS, (self.B_const,))
            self.ts("dve", self.nbfg[:], self.nbfg[:], -1.0, None, ALU.mult, None, (self.B_const,), (self.B_const,))
            self.ps = [st.enter_context(nc.psum_tensor("ps%d" % i, [128, 512], F32)) for i in range(8)]
            self.B_ps = [Buf("ps%d" % i) for i in range(8)]
            self.dsem_ld = [S.dmasem("ld%d" % i) for i in range(4)]
            self.dsem_st = [S.dmasem("st%d" % i) for i in range(6)]
            self.dsem_w = [S.dmasem("w%d" % i) for i in range(5)]
            self.dsem_misc = S.dmasem("misc")
            self.dsem_misc2 = S.dmasem("misc2")
            self.B_dram = {k: Buf(k) for k in ("hT", "qT", "kT", "vv", "qmT", "mkT", "mv", "hdT", "g8", "outT")}
            S.barrier()
            for l in range(L):
                self.reset_region("A", PB, TOTAL)
                self.phase_A(l)
                S.barrier()
                self.reset_region("B", PB, TOTAL)
                self.phase_B(l)
                S.barrier()
                self.reset_region("C", PB, TOTAL)
                self.phase_C(l)
                S.barrier()
            S.emit()
        return nc

    def load_weight(self, dst, src_rows, ncols, kchunks, eng_cycle, B_w, stage_bufs, col0=0, pw=2048):
        S = self.S
        pieces = []
        for k in range(kchunks):
            c = 0
            while c < ncols:
                w = min(pw, ncols - c)
                pieces.append((k, c, w))
                c += w
        for i, (k, c, w) in enumerate(pieces):
            stg, B_stg, dsem = stage_bufs[i % len(stage_bufs)]
            S.dma("sp", dsem, self.dmaf(stg[:, 0:w], src_rows(k)[:, col0 + c:col0 + c + w]), (), (B_stg,))
            eng = eng_cycle[i % len(eng_cycle)]
            self.copy(eng, dst[:, k, c:c + w], stg[:, 0:w], (B_stg,), (B_w,))

    def rmsnorm(self, hch, B_h, gain_ap_fn, N, xn, B_xn, sq, B_sq, psb, rstd, B_rstd, part=None):
        ps_t = self.ps[psb]
        B_p = self.B_ps[psb]
        if part in (None, 1):
            for c in range(NC8):
                self.act(sq[:, c, 0:N], hch[:, c, 0:N], AF.Square, (B_h,), (B_sq,))
        if part == 1:
            return
        for c in range(NC8):
            self.mm(ps_t[:, 0:N], self.onesm[:], sq[:, c, 0:N], c == 0, c == NC8 - 1, (B_sq, self.B_const), (B_p,))
        self.act(rstd[:, 0:N], ps_t[:, 0:N], AF.Ln, (B_p, self.B_const), (B_rstd,), bias=self.epsb[:, 0:1], scale=1.0)
        self.act(rstd[:, 0:N], rstd[:, 0:N], AF.Exp, (B_rstd,), (B_rstd,), scale=-0.5)
        for c in range(NC8):
            g = gain_ap_fn(c)
            self.S.op("dve", (lambda o, i0, sc, i1: (lambda e: e.scalar_tensor_tensor(out=o, in0=i0, scalar=sc, in1=i1, op0=ALU.mult, op1=ALU.mult)))(
                xn[:, c, 0:N], hch[:, c, 0:N], g, rstd[:, 0:N]), (B_h, B_rstd, self.B_const), (B_xn,))

    def phase_A(self, l):
        S = self.S
        SL, NT, NQ = self.SL, self.NT, self.NQ
        fox = (l % 2 == 0)
        j = l // 2
        PW = FOXP if fox else MOBAP
        wsrc = self.w_fox if fox else self.w_moba
        QM0 = 3 * SW + (NH if fox else 0)
        R = "A"
        win = self.sb([128, 8, PW], BF16, R)
        wmk = self.sb([128, 8, 2 * MW], BF16, R)
        stg = [(self.sb([128, 2048], F32, R), Buf("stg%d" % i), self.dsem_w[i]) for i in range(4)]
        hch = [self.sb([128, 8, 512], F32, R) for _ in range(2)]
        B_hch = [Buf("hch0"), Buf("hch1")]
        sq = self.sb([128, 8, 512], BF16, R)
        B_sq = Buf("sq")
        xn2 = [self.sb([128, 8, 512], BF16, R) for _ in range(2)]
        B_xn2 = [Buf("xn0"), Buf("xn1")]
        xn = xn2[0]
        B_xn = B_xn2[0]
        rstd = self.sb([128, 512], F32, R)
        B_rstd = Buf("rstd")
        ev = [self.sb([128, 512], BF16, R) for _ in range(4)]
        B_ev = [Buf("ev%d" % i) for i in range(4)]
        vst = [self.sb([128, SW], BF16, R) for _ in range(2)]
        B_vst = [Buf("vst%d" % i) for i in range(2)]
        B_win = Buf("win")
        B_wmk = Buf("wmk")
        if fox:
            gT = self.sb([NH, SL], F32, R)
            B_gT = Buf("gT")
            g8t = self.sb([NH, SL], BF16, R)
            B_g8t = Buf("g8t")
            onesrow = self.sb([NH, 512], F32, R)
            B_onesrow = Buf("onesrow")
            self.memset("dve", onesrow[:], 1.0, (B_onesrow,))
        hsrc = self.xT if l == 0 else self.hT
        self.load_weight(wmk, lambda k: self.w_mkv[l, k * 128:(k + 1) * 128, :], 2 * MW, 8, ["pool", "dve"], B_wmk, stg)
        self.load_weight(win, lambda k: wsrc[j, k * 128:(k + 1) * 128, :], PW, 8, ["pool", "dve", "act"], B_win, stg)
        evc = [0]
        vsc = [0]

        def evac_store(ps_ap, rows, N, dram_ap, B_p, dkey, use):
            i = evc[0] % 4
            evc[0] += 1
            eng = "act" if use % 2 == 0 else "dve"
            self.copy(eng, ev[i][0:rows, 0:N], ps_ap, (B_p,), (B_ev[i],))
            S.dma("pool", self.dsem_st[i], self.dmaf(dram_ap, ev[i][0:rows, 0:N]), (B_ev[i],), (self.B_dram[dkey],))

        S.dma("sp", self.dsem_ld[0], self.dmaf(hch[0][:, :, 0:NMEM], self.memT.rearrange("(c p) n -> p c n", p=128)), (), (B_hch[0],))
        self.rmsnorm(hch[0], B_hch[0], lambda c: self.gmem[:, l * 8 + c:l * 8 + c + 1], NMEM, xn, B_xn, sq, B_sq, 7, rstd, B_rstd)
        pi = 0
        for ft in range(2):
            pb = pi % 6
            pi += 1
            for c in range(NC8):
                self.mm(self.ps[pb][:, 0:NMEM], wmk[:, c, ft * 128:(ft + 1) * 128], xn[:, c, 0:NMEM], c == 0, c == 7, (B_wmk, B_xn), (self.B_ps[pb],))
            evac_store(self.ps[pb][:, 0:NMEM], 128, NMEM, self.mkT[ft * 128:(ft + 1) * 128, :], self.B_ps[pb], "mkT", ft)
        for nt in range(2):
            pb = pi % 6
            pi += 1
            for c in range(NC8):
                self.mm(self.ps[pb][:, 0:MW], xn[:, c, nt * 128:(nt + 1) * 128], wmk[:, c, MW:2 * MW], c == 0, c == 7, (B_wmk, B_xn), (self.B_ps[pb],))
            evac_store(self.ps[pb][:, 0:MW], 128, MW, self.mv[nt * 128:(nt + 1) * 128, :], self.B_ps[pb], "mv", nt)

        def load_chunk(I):
            b = I % 2
            S.dma("sp", self.dsem_ld[b], self.dmaf(hch[b][:], hsrc[:, I * 512:(I + 1) * 512].rearrange("(c p) n -> p c n", p=128)),
                  (self.B_dram["hT"],), (B_hch[b],))
        load_chunk(0)
        if NQ > 1:
            load_chunk(1)

        def norm_chunk(I, part=None):
            self.rmsnorm(hch[I % 2], B_hch[I % 2], lambda c: self.gmix[:, l * 8 + c:l * 8 + c + 1], 512, xn2[I % 2], B_xn2[I % 2], sq, B_sq, 7, rstd, B_rstd, part=part)
        norm_chunk(0)
        for I in range(NQ):
            b = I % 2
            xn = xn2[b]
            B_xn = B_xn2[b]
            t0 = I * 512
            tiles = [("qT", self.qT, ft, ft * 128) for ft in range(6)] + [("kT", self.kT, ft, SW + ft * 128) for ft in range(6)] + \
                    [("qmT", self.qmT, ft, QM0 + ft * 128) for ft in range(2)]
            for ti, (dkey, dram, ft, col) in enumerate(tiles):
                if ti == 2 and I + 1 < NQ:
                    norm_chunk(I + 1, part=1)
                if ti == 7 and I + 1 < NQ:
                    norm_chunk(I + 1, part=2)
                    if I + 2 < NQ:
                        load_chunk(I + 2)
                pb = pi % 6
                pi += 1
                for c in range(NC8):
                    self.mm(self.ps[pb][:, :], win[:, c, col:col + 128], xn[:, c, :], c == 0, c == 7, (B_win, B_xn), (self.B_ps[pb],))
                evac_store(self.ps[pb][:, :], 128, 512, dram[ft * 128:(ft + 1) * 128, t0:t0 + 512], self.B_ps[pb], dkey, pi)
            for tt_ in range(4):
                vi = vsc[0] % 2
                vsc[0] += 1
                for half, (c0, wd) in enumerate(((0, 512), (512, 256))):
                    pb = pi % 6
                    pi += 1
                    for c in range(NC8):
                        self.mm(self.ps[pb][:, 0:wd], xn[:, c, tt_ * 128:(tt_ + 1) * 128], win[:, c, 2 * SW + c0:2 * SW + c0 + wd], c == 0, c == 7,
                                (B_win, B_xn), (self.B_ps[pb],))
                    self.copy("act" if half == 0 else "dve", vst[vi][:, c0:c0 + wd], self.ps[pb][:, 0:wd], (self.B_ps[pb],), (B_vst[vi],))
                S.dma("pool", self.dsem_st[4 + vi], self.dmaf(self.vv[t0 + tt_ * 128:t0 + (tt_ + 1) * 128, :], vst[vi][:]), (B_vst[vi],), (self.B_dram["vv"],))
            if fox:
                pb = 6
                for c in range(NC8):
                    self.mm(self.ps[pb][0:NH, :], win[:, c, 3 * SW:3 * SW + NH], xn[:, c, :], c == 0, c == 7, (B_win, B_xn), (self.B_ps[pb],))
                self.act(gT[:, t0:t0 + 512], self.ps[pb][0:NH, :], AF.Exp, (self.B_ps[pb], self.B_const, B_gT), (B_gT,), bias=self.nbfg[:, j:j + 1], scale=-1.0)
                self.act(gT[:, t0:t0 + 512], gT[:, t0:t0 + 512], AF.Ln, (B_gT,), (B_gT,), bias=1.0, scale=1.0)
                init = 0.0 if I == 0 else gT[:, t0 - 1:t0]
                S.op("dve", (lambda o, d0, d1, ini: (lambda e: e.tensor_tensor_scan(out=o, data0=d0, data1=d1, initial=ini, op0=ALU.mult, op1=ALU.add)))(
                    gT[:, t0:t0 + 512], onesrow[:, :], gT[:, t0:t0 + 512], init), (B_gT, B_onesrow), (B_gT,))
        if fox:
            self.ts("dve", g8t[:], gT[:], -8.0, None, ALU.mult, None, (B_gT,), (B_g8t,))
            S.dma("pool", self.dsem_misc2, self.dmaf(self.g8[:, :], g8t[:]), (B_g8t,), (self.B_dram["g8"],))
            gview = self.gtab[:].rearrange("p (h j) -> p j h", h=NH)
            for j0 in range(0, NT, 32):
                nj = min(32, NT - j0)
                pb = (j0 // 32) % 2
                for jj in range(nj):
                    jt = j0 + jj
                    self.mm(self.ps[pb][:, jj * NH:(jj + 1) * NH], gT[:, jt * 128:(jt + 1) * 128], self.identf[0:NH, 0:NH], True, True,
                            (B_gT, self.B_const), (self.B_ps[pb],))
                self.copy("dve", gview[:, j0:j0 + nj, :], self.ps[pb][:, 0:nj * NH].rearrange("p (j h) -> p j h", h=NH), (self.B_ps[pb],), (self.B_gtab,))

    def phase_B(self, l):
        S = self.S
        SL, NT, NQ, NB = self.SL, self.NT, self.NQ, self.NB
        fox = (l % 2 == 0)
        R = "B"
        KA = 97
        LA = 2
        KT = [self.sb([128, SL], BF16, R) for _ in range(2)]
        QT = [self.sb([128, SL], BF16, R) for _ in range(2)]
        VA = [self.sb([128, NT, 72], BF16, R) for _ in range(2)]
        B_K = [Buf("K0"), Buf("K1")]
        B_Q = [Buf("Q0"), Buf("Q1")]
        B_V = [Buf("V0"), Buf("V1")]
        B_Qm = [[Buf("Qm%d_%d" % (s, I)) for I in range(NQ)] for s in range(2)]
        PT = [self.sb([128, 512], BF16, R) for _ in range(4)]
        B_PT = [Buf("PT%d" % i) for i in range(4)]
        rrow = [self.sb([128, 512], F32, R) for _ in range(2)]
        B_rrow = [Buf("rrow0"), Buf("rrow1")]
        sbB = [self.sb([64, 512], F32, R) for _ in range(2)]
        B_sbB = [Buf("sbB0"), Buf("sbB1")]
        hst = [self.sb([64, 512], BF16, R) for _ in range(2)]
        B_hst = [Buf("hst0"), Buf("hst1")]
        zb = self.sb([128, 1], F32, R)
        B_zb = Buf("zb")
        self.memset("dve", zb[:], 0.0, (B_zb,))
        if not fox:
            gm = self.sb([128, 4, 32], F32, R)
            B_gm = Buf("gm")
            m8 = self.sb([128, 4, 8], F32, R)
            B_m8 = Buf("m8")
            thr = self.sb([128, 4], F32, R)
            B_thr = Buf("thr")
            stage = [self.sb([128, 96], BF16, R) for _ in range(4)]
            B_stage = [Buf("stage%d" % i) for i in range(4)]
            kms = self.sb([64, NB], F32, R)
            kmh = self.sb([64, NB], BF16, R)
            kml = self.sb([64, NB], BF16, R)
            kmhf = self.sb([64, NB], F32, R)
            B_km = Buf("km")
            akbh = [self.sb([128, NT], F32, R) for _ in range(2)]
            B_akbh = [Buf("akbh0"), Buf("akbh1")]
            for i in range(4):
                self.memset("dve", stage[i][:], 0.0, (B_stage[i],))
        for s in range(2):
            self.memset("dve", VA[s][:, :, 64:65], 1.0, (B_V[s],))
            self.memset("pool", KT[s][96:97, :], 1.0, (B_K[s],))
            if fox:
                self.memset("pool", QT[s][64:96, :], 0.0, (B_Q[s],))
        B_Kst = Buf("kstatic")
        S.dma_group("sp", self.dsem_misc, [(self.dmaf(KT[s_][64:96, :], self.c_onehot[:, :]), (), (B_Kst,)) for s_ in range(2)])
        psS = [0, 1, 2]
        psO = [3, 4]
        psBk = 5
        psG = 6
        psTr = 7
        heads = [("self", h) for h in range(NH)] + [("mem", h) for h in range(NMH)]

        def load_head(idx):
            kind, h = heads[idx]
            s = idx % 2
            items = []
            if kind == "self":
                items.append((self.dmaf(KT[s][0:64, :], self.kT[h * 64:(h + 1) * 64, :]), (self.B_dram["kT"],), (B_K[s],)))
                items.append((self.dmaf(QT[s][0:64, :], self.qT[h * 64:(h + 1) * 64, :]), (self.B_dram["qT"],), (B_Q[s],)))
                shift = self.g8[h:h + 1, :] if fox else self.c_aqs[h:h + 1, :]
                items.append((self.dmaf(QT[s][96:97, :], shift), (self.B_dram["g8"],), (B_Q[s],)))
                items.append((self.dmaf(VA[s][:, :, 0:64], self.vv[:, h * 64:(h + 1) * 64].rearrange("(j p) d -> p j d", p=128)),
                              (self.B_dram["vv"],), (B_V[s],)))
            else:
                items.append((self.dmaf(KT[s][0:64, 0:NMEM], self.mkT[h * 64:(h + 1) * 64, :]), (self.B_dram["mkT"],), (B_K[s],)))
                items.append((self.dmaf(QT[s][0:64, :], self.qmT[h * 64:(h + 1) * 64, :]), (self.B_dram["qmT"],), (B_Q[s],)))
                items.append((self.dmaf(VA[s][:, 0:2, 0:64], self.mv[:, h * 64:(h + 1) * 64].rearrange("(j p) d -> p j d", p=128)),
                              (self.B_dram["mv"],), (B_V[s],)))
            S.dma_group("sp", self.dsem_ld[s], items)

        def kmean(idx):
            s = idx % 2
            h = heads[idx][1]
            S.op("dve", (lambda o, i: (lambda e: e.tensor_reduce(out=o, in_=i, axis=AX.X, op=ALU.add)))(
                kms[:, :], KT[s][0:64, :].rearrange("d (n s) -> d n s", s=256)), (B_K[s],), (B_km,))
            self.ts("dve", kmh[:, :], kms[:, :], 1.0 / 256.0, None, ALU.mult, None, (B_km,), (B_km,))
            self.copy("dve", kmhf[:, :], kmh[:, :], (B_km,), (B_km,))
            S.op("dve", (lambda o, i0, i1: (lambda e: e.scalar_tensor_tensor(out=o, in0=i0, scalar=1.0 / 256.0, in1=i1, op0=ALU.mult, op1=ALU.subtract)))(
                kml[:, :], kms[:, :], kmhf[:, :]), (B_km,), (B_km,))
            self.ts("dve", akbh[s][:, :], self.akb[:, h * NT:(h + 1) * NT], 1.0, None, ALU.mult, None, (self.B_const,), (B_akbh[s],))

        def gate1(idx, I):
            s = idx % 2
            pg = self.ps[psG]
            B_pg = self.B_ps[psG]
            for qi in range(4):
                qt = 4 * I + qi
                self.mm(pg[:, qi * 32:qi * 32 + NB], QT[s][0:64, qt * 128:(qt + 1) * 128], kmh[:, :], True, False, (B_Q[s], B_km), (B_pg,))
                self.mm(pg[:, qi * 32:qi * 32 + NB], QT[s][0:64, qt * 128:(qt + 1) * 128], kml[:, :], False, True, (B_Q[s], B_km), (B_pg,))
            for qi in range(4):
                own = (4 * I + qi) // 2
                self.tt("dve", gm[:, qi, 0:NB], pg[:, qi * 32:qi * 32 + NB], self.bm[:, own * 32:own * 32 + NB], ALU.add, (B_pg, self.B_const), (B_gm,))
            for qi in range(4):
                S.op("dve", (lambda o, i: (lambda e: e.max(out=o, in_=i)))(m8[:, qi, :], gm[:, qi, 0:NB]), (B_gm,), (B_m8,))
            self.ts("dve", thr[:, :], m8[:, :, 3], -1e29, None, ALU.max, None, (B_m8,), (B_thr,))
            for qi in range(4):
                self.ts("dve", stage[qi][:, 64:64 + NB], gm[:, qi, 0:NB], thr[:, qi:qi + 1], MASKV, ALU.is_lt, ALU.mult, (B_gm, B_thr), (B_stage[qi],))

        def gate2(idx, I):
            s = idx % 2
            ptr = self.ps[psTr]
            B_ptr = self.B_ps[psTr]
            for qi in range(4):
                self.mm(ptr[0:96, qi * 128:(qi + 1) * 128], stage[qi][:, :], self.identb[:, :], True, True, (B_stage[qi], self.B_const), (B_ptr,))
            self.copy("act", QT[s][64:96, I * 512:(I + 1) * 512], ptr[64:96, :], (B_ptr,), (B_Qm[s][I],))

        units = []
        for idx, (kind, h) in enumerate(heads):
            k = 0
            for I in range(NQ):
                jl = list(range(4 * I + 4)) if kind == "self" else [0, 1]
                for ji, jt in enumerate(jl):
                    units.append((idx, I, ji, jt, len(jl), k))
                    k += 1
        n = len(units)
        state = {"step": 0}
        deferred = []

        def defer(k, fn, key=None):
            deferred.append((state["step"] + k, fn, key))

        def run_due(force=False, key=None):
            for ent in deferred[:]:
                due, fn, kk = ent
                if force or due <= state["step"] or (key is not None and kk == key):
                    deferred.remove(ent)
                    fn()

        def stage12(u, uid):
            idx, I, ji, jt, nj, k = u
            kind, h = heads[idx]
            s = idx % 2
            selfh = (kind == "self")
            moba = selfh and not fox
            K = KA if selfh else 64
            t0 = I * 512
            c0 = 0
            diag = False
            if selfh and jt >= 4 * I:
                c0 = 128 * (jt - 4 * I)
                diag = True
            sb_ = psS[uid % 3]
            pi_ = uid % 4
            pS = self.ps[sb_]
            rd = [B_K[s], B_Q[s], B_Kst]
            if moba:
                rd.append(B_Qm[s][I])
            self.mm(pS[:, c0:512], KT[s][0:K, jt * 128:(jt + 1) * 128], QT[s][0:K, t0 + c0:t0 + 512], True, not diag, rd, (self.B_ps[sb_],))
            if diag:
                self.mm(pS[:, c0:c0 + 128], self.identb[:, :], self.tri[:, :], False, True, (self.B_const,), (self.B_ps[sb_],))
            if not selfh:
                bias = zb[:, 0:1]
                rb = B_zb
            elif fox:
                bias = self.gtab[:, h * NT + jt:h * NT + jt + 1]
                rb = self.B_gtab
            else:
                bias = akbh[s][:, jt:jt + 1]
                rb = B_akbh[s]
            self.act(PT[pi_][:, c0:512], pS[:, c0:512], AF.Exp, (self.B_ps[sb_], rb), (B_PT[pi_],), bias=bias, scale=0.125)

        chunk_seq = {}

        def stage3(u, uid):
            idx, I, ji, jt, nj, k = u
            kind, h = heads[idx]
            s = idx % 2
            selfh = (kind == "self")
            t0 = I * 512
            c0 = 0
            if selfh and jt >= 4 * I:
                c0 = 128 * (jt - 4 * I)
            cs = idx * NQ + I
            o = psO[cs % 2]
            po = self.ps[o]
            B_po = self.B_ps[o]
            pi_ = uid % 4
            if ji == 0:
                run_due(key=("fin", o))
            self.mm(po[0:65, c0:512], VA[s][:, jt, 0:65], PT[pi_][:, c0:512], ji == 0, ji == nj - 1, (B_V[s], B_PT[pi_]), (B_po,))
            if k == 0 and idx + 1 < len(heads):
                load_head(idx + 1)
            if ji == nj - 1:
                hi = cs % 2
                S.op("dve", (lambda o_, i_: (lambda e: e.reciprocal(out=o_, in_=i_)))(rrow[hi][64:65, :], po[64:65, :]), (B_po,), (B_rrow[hi],))

                def fin(hi=hi, po=po, B_po=B_po, h=h, selfh=selfh, t0=t0):
                    pb_ = self.ps[psBk]
                    self.mm(pb_[0:64, :], self.onesf[64:65, 0:64], rrow[hi][64:65, :], True, True, (B_rrow[hi], self.B_const), (self.B_ps[psBk],))
                    self.copy("dve", sbB[hi][:, :], pb_[0:64, :], (self.B_ps[psBk],), (B_sbB[hi],))
                    self.tt("dve", hst[hi][:, :], po[0:64, :], sbB[hi][:, :], ALU.mult, (B_po, B_sbB[hi]), (B_hst[hi],))
                    row0 = h * 64 if selfh else SW + h * 64
                    S.dma("pool", self.dsem_st[hi], self.dmaf(self.hdT[row0:row0 + 64, t0:t0 + 512], hst[hi][:, :]), (B_hst[hi],), (self.B_dram["hdT"],))
                defer(12, fin, key=("fin", o))

        nhu = sum(4 * I + 4 for I in range(NQ))
        k_km = max(LA + 1, min(64, nhu // 8))
        k_g0 = k_km + max(1, min(16, nhu // 16))
        g_stride = max(1, min(24, (nhu - k_g0 - 12) // NQ))
        load_head(0)
        if not fox:
            kmean(0)
            for I in range(NQ):
                gate1(0, I)
                gate2(0, I)
        for step in range(n + LA):
            state["step"] = step
            if step < n:
                u = units[step]
                idx, I, ji, jt, nj, k = u
                if (not fox) and idx + 1 < NH:
                    if k == k_km:
                        kmean(idx + 1)
                    g, r = divmod(k - k_g0, g_stride)
                    if k >= k_g0 and r == 0 and g < NQ:
                        run_due(key="gate2")
                        gate1(idx + 1, g)
                        defer(10, (lambda a, b: (lambda: gate2(a, b)))(idx + 1, g), key="gate2")
                stage12(u, step)
            if step - LA >= 0:
                stage3(units[step - LA], step - LA)
            run_due()
        run_due(force=True)

    def phase_C(self, l):
        S = self.S
        SL = self.SL
        R = "C"
        N = 256
        NCH = SL // N
        last = (l == self.depth - 1)
        wo = self.sb([128, 8, D], BF16, R)
        wgu = self.sb([128, 8, 2 * DFF], BF16, R)
        wdn = self.sb([128, 22, D], BF16, R)
        B_wo, B_wgu, B_wdn = Buf("wo"), Buf("wgu"), Buf("wdn")
        stg = [(self.sb([128, 1024], F32, R), Buf("stgc%d" % i), self.dsem_w[i]) for i in range(2)]
        hch = [self.sb([128, 8, N], F32, R) for _ in range(2)]
        B_hch = [Buf("hc0"), Buf("hc1")]
        hdc1 = self.sb([128, 8, N], BF16, R)
        hdc = [hdc1, hdc1]
        B_hd1 = Buf("hd")
        B_hdc = [B_hd1, B_hd1]
        hn = self.sb([128, 8, N], BF16, R)
        B_hn = Buf("hn")
        sq = hn
        B_sq = B_hn
        rstd = self.sb([128, N], F32, R)
        B_rstd = Buf("rstdc")
        aT_off = self.off[R]
        aT = self.sb([128, 22, N], BF16, R)
        B_aT = Buf("aT")
        self.nt += 1
        stg_al = self.nc.alloc_sbuf_tensor_at("t%d" % self.nt, [128, 2, 1024], F32, offset=aT_off)
        stg_big = stg + [(stg_al[:, 0, :], Buf("stga0"), self.dsem_w[2]), (stg_al[:, 1, :], Buf("stga1"), self.dsem_w[3])]
        gact = [self.sb([128, N], F32, R) for _ in range(2)]
        B_gact = [Buf("ga%d" % i) for i in range(2)]
        hsrc = self.xT if l == 0 else self.hT
        self.load_weight(wo, lambda k: self.w_out[l, k * 128:(k + 1) * 128, :], D, 8, ["pool", "dve", "act"], B_wo, stg_big, pw=1024)
        self.load_weight(wgu, lambda k: self.w_gu[l, k * 128:(k + 1) * 128, :], 2 * DFF, 8, ["pool", "dve", "act"], B_wgu, stg_big, pw=1024)
        self.load_weight(wdn, lambda k: self.w_dn[l, k * 128:(k + 1) * 128, :], D, 22, ["pool", "dve", "act"], B_wdn, stg, pw=1024)

        def load_chunk(I):
            b = I % 2
            S.dma("sp", self.dsem_ld[b], self.dmaf(hch[b][:], hsrc[:, I * N:(I + 1) * N].rearrange("(c p) n -> p c n", p=128)),
                  (self.B_dram["hT"],), (B_hch[b],))

        def load_hd(I):
            b = I % 2
            S.dma("sp", self.dsem_ld[2 + b], self.dmaf(hdc[b][:], self.hdT[:, I * N:(I + 1) * N].rearrange("(c p) n -> p c n", p=128)),
                  (self.B_dram["hdT"],), (B_hdc[b],))
        load_chunk(0)
        load_hd(0)
        if NCH > 1:
            load_chunk(1)
        st_ = {"pi": 0, "ga": 0}

        def outproj(I):
            b = I % 2
            for fo in range(8):
                pb = st_["pi"] % 7
                st_["pi"] += 1
                for c in range(8):
                    self.mm(self.ps[pb][:, 0:N], wo[:, c, fo * 128:(fo + 1) * 128], hdc[b][:, c, :], c == 0, c == 7, (B_wo, B_hdc[b]), (self.B_ps[pb],))
                self.tt("dve", hch[b][:, fo, :], self.ps[pb][:, 0:N], hch[b][:, fo, :], ALU.add, (self.B_ps[pb], B_hch[b]), (B_hch[b],))
            if I + 1 < NCH:
                load_hd(I + 1)

        def norm(I):
            b = I % 2
            self.rmsnorm(hch[b], B_hch[b], lambda c: self.gffn[:, l * 8 + c:l * 8 + c + 1], N, hn, B_hn, sq, B_sq, 7, rstd, B_rstd)

        def down(I, fos):
            b = I % 2
            for fo in fos:
                pb = st_["pi"] % 7
                st_["pi"] += 1
                for k in range(22):
                    self.mm(self.ps[pb][:, 0:N], wdn[:, k, fo * 128:(fo + 1) * 128], aT[:, k, :], k == 0, k == 21, (B_wdn, B_aT), (self.B_ps[pb],))
                self.tt("dve", hch[b][:, fo, :], self.ps[pb][:, 0:N], hch[b][:, fo, :], ALU.add, (self.B_ps[pb], B_hch[b]), (B_hch[b],))

        outproj(0)
        norm(0)
        for I in range(NCH):
            b = I % 2
            t0 = I * N
            for ft in range(22):
                pg = st_["pi"] % 7
                st_["pi"] += 1
                for c in range(8):
                    self.mm(self.ps[pg][:, 0:N], wgu[:, c, ft * 128:(ft + 1) * 128], hn[:, c, :], c == 0, c == 7, (B_wgu, B_hn), (self.B_ps[pg],))
                gi = st_["ga"] % 2
                st_["ga"] += 1
                self.act(gact[gi][:, :], self.ps[pg][:, 0:N], AF.Silu, (self.B_ps[pg],), (B_gact[gi],))
                pu = st_["pi"] % 7
                st_["pi"] += 1
                for c in range(8):
                    self.mm(self.ps[pu][:, 0:N], wgu[:, c, DFF + ft * 128:DFF + (ft + 1) * 128], hn[:, c, :], c == 0, c == 7, (B_wgu, B_hn), (self.B_ps[pu],))
                self.tt("dve", aT[:, ft, :], self.ps[pu][:, 0:N], gact[gi][:, :], ALU.mult, (self.B_ps[pu], B_gact[gi]), (B_aT,))
            if I + 1 < NCH:
                outproj(I + 1)
            down(I, range(0, 4))
            if I + 1 < NCH:
                norm(I + 1)
            down(I, range(4, 8))
            if not last:
                S.dma("pool", self.dsem_st[b], self.dmaf(self.hT[:, t0:t0 + N].rearrange("(c p) n -> p c n", p=128), hch[b][:]),
                      (B_hch[b],), (self.B_dram["hT"],))
            else:
                self.final_norm(hch[b], B_hch[b], N, aT, B_aT, rstd, B_rstd)
                S.dma("pool", self.dsem_st[b], self.dmaf(self.outT[:, t0:t0 + N].rearrange("(c p) n -> p c n", p=128), hch[b][:]),
                      (B_hch[b],), (self.B_dram["outT"],))
            if I + 2 < NCH:
                load_chunk(I + 2)

    def final_norm(self, hch, B_h, N, sq, B_sq, rstd, B_rstd):
        ps_t = self.ps[7]
        B_p = self.B_ps[7]
        for c in range(NC8):
            self.act(sq[:, c, 0:N], hch[:, c, 0:N], AF.Square, (B_h,), (B_sq,))
        for c in range(NC8):
            self.mm(ps_t[:, 0:N], self.onesm[:], sq[:, c, 0:N], c == 0, c == NC8 - 1, (B_sq, self.B_const), (B_p,))
        self.act(rstd[:, 0:N], ps_t[:, 0:N], AF.Ln, (B_p, self.B_const), (B_rstd,), bias=self.epsb[:, 0:1], scale=1.0)
        self.act(rstd[:, 0:N], rstd[:, 0:N], AF.Exp, (B_rstd,), (B_rstd,), scale=-0.5)
        for c in range(NC8):
            self.S.op("dve", (lambda o, i0, sc, i1: (lambda e: e.scalar_tensor_tensor(out=o, in0=i0, scalar=sc, in1=i1, op0=ALU.mult, op1=ALU.mult)))(
                hch[:, c, 0:N], hch[:, c, 0:N], self.gfin[:, c:c + 1], rstd[:, 0:N]), (B_h, B_rstd, self.B_const), (B_h,))


def _consts(SL):
    NT = SL // 128
    NB = SL // 256
    bf = ml_dtypes.bfloat16
    c = {}
    c["c_identb"] = np.eye(128, dtype=np.float32).astype(bf)
    c["c_identf"] = np.eye(128, dtype=np.float32)
    s = np.arange(128)
    c["c_tri"] = np.where(s[None, :] >= s[:, None], 0.0, MASKV).astype(np.float32).astype(bf)
    pos = np.arange(SL)
    c["c_onehot"] = (pos[None, :] // 256 == np.arange(32)[:, None]).astype(np.float32).astype(bf)
    slopes = (2.0 ** (-8.0 * np.arange(1, NH + 1, dtype=np.float32) / NH)).astype(np.float32)
    p = np.arange(128, dtype=np.float32)
    jj = np.arange(NT, dtype=np.float32)
    kb = slopes[None, :, None] * (128.0 * jj[None, None, :] + p[:, None, None])
    c["c_alibi_kbias"] = kb.reshape(128, NH * NT).astype(np.float32)
    c["c_alibi_qshift"] = (-8.0 * slopes[:, None] * pos[None, :].astype(np.float32)).astype(np.float32).astype(bf)
    bm = np.zeros((NB, 32), np.float32)
    for own in range(NB):
        bm[own, own] = 1e30
        bm[own, own + 1:] = -1e30
    c["c_moba_biasmask"] = np.ascontiguousarray(np.broadcast_to(bm.reshape(1, NB * 32), (128, NB * 32))).astype(np.float32)
    return c


def _gain_layout(g):
    L = g.shape[0]
    return np.ascontiguousarray(g.reshape(L, 8, 128).transpose(2, 0, 1).reshape(128, L * 8)).astype(np.float32)


_CACHE = {}


def run(x, mem, norm_mix, norm_mem, norm_ffn, norm_final, w_in_fox, b_fgate, w_in_moba, w_mem_kv, w_out,
        w_gate_up, w_down, n_cores=8):
    B, SL, _ = x.shape
    depth = norm_mix.shape[0]
    key = (SL, depth)
    if key not in _CACHE:
        _CACHE[key] = Prog(SL, depth).build()
    nc = _CACHE[key]
    consts = _consts(SL)
    shared = dict(consts)
    shared["g_mix"] = _gain_layout(np.asarray(norm_mix))
    shared["g_mem"] = _gain_layout(np.asarray(norm_mem))
    shared["g_ffn"] = _gain_layout(np.asarray(norm_ffn))
    shared["g_fin"] = _gain_layout(np.asarray(norm_final)[None, :])
    shared["w_in_fox"] = np.ascontiguousarray(w_in_fox, dtype=np.float32)
    shared["b_fgate"] = np.ascontiguousarray(np.asarray(b_fgate).T, dtype=np.float32)
    wm = np.asarray(w_in_moba, dtype=np.float32)
    if wm.shape[0] == 0:
        wm = np.zeros((1, D, MOBAP), np.float32)
    shared["w_in_moba"] = np.ascontiguousarray(wm)
    shared["w_mem_kv"] = np.ascontiguousarray(w_mem_kv, dtype=np.float32)
    shared["w_out"] = np.ascontiguousarray(w_out, dtype=np.float32)
    shared["w_gate_up"] = np.ascontiguousarray(w_gate_up, dtype=np.float32)
    shared["w_down"] = np.ascontiguousarray(w_down, dtype=np.float32)
    work = [0, 1, 4, 5][:B] if n_cores == 8 else list(range(B))
    idle = None
    in_maps = []
    for cidx in range(n_cores):
        if cidx in work:
            b = work.index(cidx)
            m = dict(shared)
            m["xT"] = np.ascontiguousarray(np.asarray(x[b]).T, dtype=np.float32)
            m["memT"] = np.ascontiguousarray(np.asarray(mem[b]).T, dtype=np.float32)
        else:
            if idle is None:
                idle = {k: (v if k.startswith("c_") else np.zeros_like(v)) for k, v in shared.items()}
                idle["xT"] = np.zeros((D, SL), np.float32)
                idle["memT"] = np.zeros((D, NMEM), np.float32)
            m = idle
        in_maps.append(m)
    res = run_bass_kernel_spmd(nc, in_maps, core_ids=list(range(n_cores)))
    out = np.empty((B, SL, D), np.float32)
    for b in range(B):
        out[b] = res.results[work[b]]["outT"].T
    return out


def kernel(x, mem, norm_mix, norm_mem, norm_ffn, norm_final, w_in_fox, b_fgate, w_in_moba, w_mem_kv, w_out,
           w_gate_up, w_down):
    return run(x, mem, norm_mix, norm_mem, norm_ffn, norm_final, w_in_fox, b_fgate, w_in_moba, w_mem_kv, w_out,
               w_gate_up, w_down, n_cores=8)
```

```python
import numpy as np
import ml_dtypes
from contextlib import ExitStack
import concourse.bass as bass
import concourse.mybir as mybir
from concourse.bass_utils import run_bass_kernel_spmd

F32 = mybir.dt.float32
BF16 = mybir.dt.bfloat16
ALU = mybir.AluOpType
AF = mybir.ActivationFunctionType
AX = mybir.AxisListType

D = 1024
NH = 12
NMH = 4
HD = 64
SW = 768
MW = 256
NMEM = 256
DFF = 2816
FOXP = 2572
MOBAP = 2560
EPS = 1e-6
MASKV = -29952.0
NC8 = 8

COMPUTE = ("pe", "act", "dve", "pool")


class Buf:
    __slots__ = ("name", "lw", "rd")

    def __init__(self, name=""):
        self.name = name
        self.lw = None
        self.rd = []


class DmaSem:
    __slots__ = ("sem", "n")

    def __init__(self, sem):
        self.sem = sem
        self.n = 0


class Sched:
    def __init__(self, nc, stack):
        self.nc = nc
        self.stack = stack
        self.q = {e: [] for e in ("pe", "act", "dve", "pool", "sp")}
        self.psem = {}
        self.cnt = {}
        for e in COMPUTE:
            self.psem[e] = stack.enter_context(nc.semaphore("prog_" + e))
            self.cnt[e] = 0
        self.waited = {e: {} for e in self.q}
        self.dsems = []
        self.ninstr = 0

    def dmasem(self, name):
        s = self.stack.enter_context(self.nc.semaphore("d_" + name))
        d = DmaSem(s)
        self.dsems.append(d)
        return d

    def _deps(self, eng, reads, writes):
        deps = []
        for r in reads:
            if r.lw is not None:
                deps.append(r.lw)
        for w in writes:
            if w.lw is not None:
                deps.append(w.lw)
            deps.extend(w.rd)
        need = {}
        for (sem, val, teng) in deps:
            if eng == "pe" and teng == "pe":
                continue
            k = id(sem)
            if k not in need or need[k][1] < val:
                need[k] = (sem, val)
        waits = []
        wd = self.waited[eng]
        for k, (sem, val) in need.items():
            if wd.get(k, 0) >= val:
                continue
            wd[k] = val
            waits.append((sem, val))
        return waits

    @staticmethod
    def _commit(tok, reads, writes):
        for r in reads:
            r.rd.append(tok)
        for w in writes:
            w.lw = tok
            w.rd = []

    def op(self, eng, fn, reads=(), writes=()):
        waits = self._deps(eng, reads, writes)
        self.cnt[eng] += 1
        tok = (self.psem[eng], self.cnt[eng], eng)
        self.q[eng].append((waits, fn, (self.psem[eng], 1)))
        self._commit(tok, reads, writes)
        self.ninstr += 1
        return tok

    def dma_group(self, q, dsem, items):
        for fn, reads, writes in items:
            waits = self._deps(q, reads, writes)
            dsem.n += 1
            self.q[q].append((waits, fn, (dsem.sem, 16)))
            self.ninstr += 1
        tok = (dsem.sem, 16 * dsem.n, "dma")
        for fn, reads, writes in items:
            self._commit(tok, reads, writes)
        return tok

    def dma(self, q, dsem, fn, reads=(), writes=()):
        return self.dma_group(q, dsem, [(fn, reads, writes)])

    def barrier(self):
        for e in self.q:
            waits = []
            wd = self.waited[e]
            for o in COMPUTE:
                if o == e or self.cnt[o] == 0:
                    continue
                k = id(self.psem[o])
                if wd.get(k, 0) < self.cnt[o]:
                    wd[k] = self.cnt[o]
                    waits.append((self.psem[o], self.cnt[o]))
            for d in self.dsems:
                if d.n == 0:
                    continue
                k = id(d.sem)
                if wd.get(k, 0) < 16 * d.n:
                    wd[k] = 16 * d.n
                    waits.append((d.sem, 16 * d.n))
            if waits:
                self.q[e].append((waits, None, None))

    def emit(self):
        nc = self.nc
        q = self.q

        def run(engobj, lst):
            for waits, fn, inc in lst:
                for sem, val in waits:
                    engobj.wait_ge(sem, val)
                if fn is not None:
                    ins = fn(engobj)
                    ins.then_inc(inc[0], inc[1])

        with nc.Block() as block:
            @block.sync
            def _(e):
                run(e, q["sp"])

            @block.tensor
            def _(e):
                run(e, q["pe"])

            @block.scalar
            def _(e):
                run(e, q["act"])

            @block.vector
            def _(e):
                run(e, q["dve"])

            @block.gpsimd
            def _(e):
                run(e, q["pool"])


class Prog:
    def __init__(self, S_len, depth):
        self.SL = S_len
        self.depth = depth
        self.NT = S_len // 128
        self.NQ = S_len // 512
        self.NB = S_len // 256

    def mm(self, out, lhsT, rhs, start, stop, reads, writes):
        return self.S.op("pe", lambda e: e.matmul(out, lhsT=lhsT, rhs=rhs, start=start, stop=stop), reads, writes)

    def act(self, out, in_, func, reads, writes, bias=None, scale=None, eng="act"):
        kw = {}
        if bias is not None:
            kw["bias"] = bias
        if scale is not None:
            kw["scale"] = scale
        return self.S.op("act", lambda e: e.activation(out=out, in_=in_, func=func, **kw), reads, writes)

    def copy(self, eng, out, in_, reads, writes):
        if eng == "act":
            return self.S.op("act", lambda e: e.copy(out=out, in_=in_), reads, writes)
        return self.S.op(eng, lambda e: e.tensor_copy(out=out, in_=in_), reads, writes)

    def tt(self, eng, out, in0, in1, op, reads, writes):
        return self.S.op(eng, lambda e: e.tensor_tensor(out=out, in0=in0, in1=in1, op=op), reads, writes)

    def ts(self, eng, out, in0, s1, s2, op0, op1, reads, writes):
        if op1 is None:
            return self.S.op(eng, lambda e: e.tensor_scalar(out=out, in0=in0, scalar1=s1, scalar2=None, op0=op0), reads, writes)
        return self.S.op(eng, lambda e: e.tensor_scalar(out=out, in0=in0, scalar1=s1, scalar2=s2, op0=op0, op1=op1), reads, writes)

    def memset(self, eng, ap, val, writes):
        return self.S.op(eng, lambda e: e.memset(ap, val), (), writes)

    def dmaf(self, out, in_):
        return lambda e: e.dma_start(out=out, in_=in_)

    def sb(self, shape, dt, region):
        nbytes = int(np.prod(shape[1:])) * (4 if dt == F32 else 2)
        nbytes = (nbytes + 63) // 64 * 64
        off = self.off[region]
        self.off[region] = off + nbytes
        assert self.off[region] <= self.lim[region], (region, self.off[region], self.lim[region])
        self.nt += 1
        return self.nc.alloc_sbuf_tensor_at("t%d" % self.nt, list(shape), dt, offset=off)

    def reset_region(self, region, base, lim):
        self.off[region] = base
        self.lim[region] = lim

    def build(self):
        SL, NT, NQ, NB = self.SL, self.NT, self.NQ, self.NB
        nc = bass.Bass("TRN2", target_bir_lowering=False)
        self.nc = nc
        dt_in = lambda name, shape, dt=F32: nc.dram_tensor(name, list(shape), dt, kind="ExternalInput").ap()
        dscr = lambda name, shape, dt: nc.dram_tensor(name, list(shape), dt, kind="Internal").ap()
        L = self.depth
        NF = (L + 1) // 2
        NM = L // 2
        self.xT = dt_in("xT", [D, SL])
        self.memT = dt_in("memT", [D, NMEM])
        self.g_mix = dt_in("g_mix", [128, L * 8])
        self.g_mem = dt_in("g_mem", [128, L * 8])
        self.g_ffn = dt_in("g_ffn", [128, L * 8])
        self.g_fin = dt_in("g_fin", [128, 8])
        self.w_fox = dt_in("w_in_fox", [NF, D, FOXP])
        self.bfg = dt_in("b_fgate", [NH, NF])
        self.w_moba = dt_in("w_in_moba", [max(NM, 1), D, MOBAP])
        self.w_mkv = dt_in("w_mem_kv", [L, D, 2 * MW])
        self.w_out = dt_in("w_out", [L, D, D])
        self.w_gu = dt_in("w_gate_up", [L, D, 2 * DFF])
        self.w_dn = dt_in("w_down", [L, DFF, D])
        self.c_identb = dt_in("c_identb", [128, 128], BF16)
        self.c_identf = dt_in("c_identf", [128, 128])
        self.c_tri = dt_in("c_tri", [128, 128], BF16)
        self.c_onehot = dt_in("c_onehot", [32, SL], BF16)
        self.c_akb = dt_in("c_alibi_kbias", [128, NH * NT])
        self.c_aqs = dt_in("c_alibi_qshift", [NH, SL], BF16)
        self.c_bm = dt_in("c_moba_biasmask", [128, NB * 32])
        self.outT = nc.dram_tensor("outT", [D, SL], F32, kind="ExternalOutput").ap()
        self.hT = dscr("hT", [D, SL], F32)
        self.qT = dscr("qT", [SW, SL], BF16)
        self.kT = dscr("kT", [SW, SL], BF16)
        self.vv = dscr("vv", [SL, SW], BF16)
        self.qmT = dscr("qmT", [MW, SL], BF16)
        self.mkT = dscr("mkT", [MW, NMEM], BF16)
        self.mv = dscr("mv", [NMEM, MW], BF16)
        self.hdT = dscr("hdT", [D, SL], BF16)
        self.g8 = dscr("g8", [NH, SL], BF16)

        with ExitStack() as st:
            S = Sched(nc, st)
            self.S = S
            self.nt = 0
            self.off = {}
            self.lim = {}
            TOTAL = 229376
            self.reset_region("P", 16640, 16640 + 12800)
            PB = 16640 + 12800
            P = self
            self.identb = self.sb([128, 128], BF16, "P")
            self.identf = self.sb([128, 128], F32, "P")
            self.tri = self.sb([128, 128], BF16, "P")
            self.onesm = self.sb([128, 128], BF16, "P")
            self.onesf = self.sb([128, 64], F32, "P")
            self.gmix = self.sb([128, L * 8], F32, "P")
            self.gmem = self.sb([128, L * 8], F32, "P")
            self.gffn = self.sb([128, L * 8], F32, "P")
            self.gfin = self.sb([128, 8], F32, "P")
            self.nbfg = self.sb([NH, NF], F32, "P")
            self.akb = self.sb([128, NH * NT], F32, "P")
            self.bm = self.sb([128, NB * 32], F32, "P")
            self.gtab = self.sb([128, NH * NT], F32, "P")
            self.epsb = self.sb([128, 1], F32, "P")
            self.B_const = Buf("const")
            self.B_gtab = Buf("gtab")
            csem = S.dmasem("const")
            items = []
            for dst, src in ((self.identb, self.c_identb), (self.identf, self.c_identf), (self.tri, self.c_tri),
                             (self.gmix, self.g_mix), (self.gmem, self.g_mem), (self.gffn, self.g_ffn),
                             (self.gfin, self.g_fin), (self.nbfg, self.bfg), (self.akb, self.c_akb),
                             (self.bm, self.c_bm)):
                items.append((self.dmaf(dst[:], src), (), (self.B_const,)))
            S.dma_group("sp", csem, items)
            self.memset("dve", self.onesm[:], 1.0 / 1024.0, (self.B_const,))
            self.memset("dve", self.onesf[:], 1.0, (self.B_const,))
            self.memset("dve", self.epsb[:], EPS, (self.B_const,))
            self.ts("dve", self.nbfg[:], self.nbfg[:], -1.0, None, ALU.mult, None, (self.B_const,), (self.B_const,))
            self.ps = [st.enter_context(nc.psum_tensor("ps%d" % i, [128, 512], F32)) for i in range(8)]
            self.B_ps = [Buf("ps%d" % i) for i in range(8)]
            self.dsem_ld = [S.dmasem("ld%d" % i) for i in range(4)]
            self.dsem_st = [S.dmasem("st%d" % i) for i in range(6)]
            self.dsem_w = [S.dmasem("w%d" % i) for i in range(5)]
            self.dsem_misc = S.dmasem("misc")
            self.dsem_misc2 = S.dmasem("misc2")
            self.B_dram = {k: Buf(k) for k in ("hT", "qT", "kT", "vv", "qmT", "mkT", "mv", "hdT", "g8", "outT")}
            S.barrier()
            for l in range(L):
                self.reset_region("A", PB, TOTAL)
                self.phase_A(l)
                S.barrier()
                self.reset_region("B", PB, TOTAL)
                self.phase_B(l)
                S.barrier()
                self.reset_region("C", PB, TOTAL)
                self.phase_C(l)
                S.barrier()
            S.emit()
        return nc

    def load_weight(self, dst, src_rows, ncols, kchunks, eng_cycle, B_w, stage_bufs, col0=0, pw=2048):
        S = self.S
        pieces = []
        for k in range(kchunks):
            c = 0
            while c < ncols:
                w = min(pw, ncols - c)
                pieces.append((k, c, w))
                c += w
        for i, (k, c, w) in enumerate(pieces):
            stg, B_stg, dsem = stage_bufs[i % len(stage_bufs)]
            S.dma("sp", dsem, self.dmaf(stg[:, 0:w], src_rows(k)[:, col0 + c:col0 + c + w]), (), (B_stg,))
            eng = eng_cycle[i % len(eng_cycle)]
            self.copy(eng, dst[:, k, c:c + w], stg[:, 0:w], (B_stg,), (B_w,))

    def rmsnorm(self, hch, B_h, gain_ap_fn, N, xn, B_xn, sq, B_sq, psb, rstd, B_rstd, part=None):
        ps_t = self.ps[psb]
        B_p = self.B_ps[psb]
        if part in (None, 1):
            for c in range(NC8):
                self.act(sq[:, c, 0:N], hch[:, c, 0:N], AF.Square, (B_h,), (B_sq,))
        if part == 1:
            return
        for c in range(NC8):
            self.mm(ps_t[:, 0:N], self.onesm[:], sq[:, c, 0:N], c == 0, c == NC8 - 1, (B_sq, self.B_const), (B_p,))
        self.act(rstd[:, 0:N], ps_t[:, 0:N], AF.Ln, (B_p, self.B_const), (B_rstd,), bias=self.epsb[:, 0:1], scale=1.0)
        self.act(rstd[:, 0:N], rstd[:, 0:N], AF.Exp, (B_rstd,), (B_rstd,), scale=-0.5)
        for c in range(NC8):
            g = gain_ap_fn(c)
            self.S.op("dve", (lambda o, i0, sc, i1: (lambda e: e.scalar_tensor_tensor(out=o, in0=i0, scalar=sc, in1=i1, op0=ALU.mult, op1=ALU.mult)))(
                xn[:, c, 0:N], hch[:, c, 0:N], g, rstd[:, 0:N]), (B_h, B_rstd, self.B_const), (B_xn,))

    def phase_A(self, l):
        S = self.S
        SL, NT, NQ = self.SL, self.NT, self.NQ
        fox = (l % 2 == 0)
        j = l // 2
        PW = FOXP if fox else MOBAP
        wsrc = self.w_fox if fox else self.w_moba
        QM0 = 3 * SW + (NH if fox else 0)
        R = "A"
        win = self.sb([128, 8, PW], BF16, R)
        wmk = self.sb([128, 8, 2 * MW], BF16, R)
        stg = [(self.sb([128, 2048], F32, R), Buf("stg%d" % i), self.dsem_w[i]) for i in range(4)]
        hch = [self.sb([128, 8, 512], F32, R) for _ in range(2)]
        B_hch = [Buf("hch0"), Buf("hch1")]
        sq = self.sb([128, 8, 512], BF16, R)
        B_sq = Buf("sq")
        xn2 = [self.sb([128, 8, 512], BF16, R) for _ in range(2)]
        B_xn2 = [Buf("xn0"), Buf("xn1")]
        xn = xn2[0]
        B_xn = B_xn2[0]
        rstd = self.sb([128, 512], F32, R)
        B_rstd = Buf("rstd")
        ev = [self.sb([128, 512], BF16, R) for _ in range(4)]
        B_ev = [Buf("ev%d" % i) for i in range(4)]
        vst = [self.sb([128, SW], BF16, R) for _ in range(2)]
        B_vst = [Buf("vst%d" % i) for i in range(2)]
        B_win = Buf("win")
        B_wmk = Buf("wmk")
        if fox:
            gT = self.sb([NH, SL], F32, R)
            B_gT = Buf("gT")
            g8t = self.sb([NH, SL], BF16, R)
            B_g8t = Buf("g8t")
            onesrow = self.sb([NH, 512], F32, R)
            B_onesrow = Buf("onesrow")
            self.memset("dve", onesrow[:], 1.0, (B_onesrow,))
        hsrc = self.xT if l == 0 else self.hT
        self.load_weight(wmk, lambda k: self.w_mkv[l, k * 128:(k + 1) * 128, :], 2 * MW, 8, ["pool", "dve"], B_wmk, stg)
        self.load_weight(win, lambda k: wsrc[j, k * 128:(k + 1) * 128, :], PW, 8, ["dve", "act", "dve", "act", "pool"], B_win, stg)
        evc = [0]
        vsc = [0]

        def evac_store(ps_ap, rows, N, dram_ap, B_p, dkey, use):
            i = evc[0] % 4
            evc[0] += 1
            eng = "act" if use % 2 == 0 else "dve"
            self.copy(eng, ev[i][0:rows, 0:N], ps_ap, (B_p,), (B_ev[i],))
            S.dma("pool", self.dsem_st[i], self.dmaf(dram_ap, ev[i][0:rows, 0:N]), (B_ev[i],), (self.B_dram[dkey],))

        S.dma("sp", self.dsem_ld[0], self.dmaf(hch[0][:, :, 0:NMEM], self.memT.rearrange("(c p) n -> p c n", p=128)), (), (B_hch[0],))
        self.rmsnorm(hch[0], B_hch[0], lambda c: self.gmem[:, l * 8 + c:l * 8 + c + 1], NMEM, xn, B_xn, sq, B_sq, 7, rstd, B_rstd)
        pi = 0
        for ft in range(2):
            pb = pi % 6
            pi += 1
            for c in range(NC8):
                self.mm(self.ps[pb][:, 0:NMEM], wmk[:, c, ft * 128:(ft + 1) * 128], xn[:, c, 0:NMEM], c == 0, c == 7, (B_wmk, B_xn), (self.B_ps[pb],))
            evac_store(self.ps[pb][:, 0:NMEM], 128, NMEM, self.mkT[ft * 128:(ft + 1) * 128, :], self.B_ps[pb], "mkT", ft)
        for nt in range(2):
            pb = pi % 6
            pi += 1
            for c in range(NC8):
                self.mm(self.ps[pb][:, 0:MW], xn[:, c, nt * 128:(nt + 1) * 128], wmk[:, c, MW:2 * MW], c == 0, c == 7, (B_wmk, B_xn), (self.B_ps[pb],))
            evac_store(self.ps[pb][:, 0:MW], 128, MW, self.mv[nt * 128:(nt + 1) * 128, :], self.B_ps[pb], "mv", nt)

        def load_chunk(I):
            b = I % 2
            S.dma("sp", self.dsem_ld[b], self.dmaf(hch[b][:], hsrc[:, I * 512:(I + 1) * 512].rearrange("(c p) n -> p c n", p=128)),
                  (self.B_dram["hT"],), (B_hch[b],))
        load_chunk(0)
        if NQ > 1:
            load_chunk(1)

        def norm_chunk(I, part=None):
            self.rmsnorm(hch[I % 2], B_hch[I % 2], lambda c: self.gmix[:, l * 8 + c:l * 8 + c + 1], 512, xn2[I % 2], B_xn2[I % 2], sq, B_sq, 7, rstd, B_rstd, part=part)
        norm_chunk(0)
        for I in range(NQ):
            b = I % 2
            xn = xn2[b]
            B_xn = B_xn2[b]
            t0 = I * 512
            tiles = [("qT", self.qT, ft, ft * 128) for ft in range(6)] + [("kT", self.kT, ft, SW + ft * 128) for ft in range(6)] + \
                    [("qmT", self.qmT, ft, QM0 + ft * 128) for ft in range(2)]
            for ti, (dkey, dram, ft, col) in enumerate(tiles):
                if ti == 2 and I + 1 < NQ:
                    norm_chunk(I + 1, part=1)
                if ti == 7 and I + 1 < NQ:
                    norm_chunk(I + 1, part=2)
                    if I + 2 < NQ:
                        load_chunk(I + 2)
                pb = pi % 6
                pi += 1
                for c in range(NC8):
                    self.mm(self.ps[pb][:, :], win[:, c, col:col + 128], xn[:, c, :], c == 0, c == 7, (B_win, B_xn), (self.B_ps[pb],))
                evac_store(self.ps[pb][:, :], 128, 512, dram[ft * 128:(ft + 1) * 128, t0:t0 + 512], self.B_ps[pb], dkey, pi)
            for tt_ in range(4):
                vi = vsc[0] % 2
                vsc[0] += 1
                for half, (c0, wd) in enumerate(((0, 512), (512, 256))):
                    pb = pi % 6
                    pi += 1
                    for c in range(NC8):
                        self.mm(self.ps[pb][:, 0:wd], xn[:, c, tt_ * 128:(tt_ + 1) * 128], win[:, c, 2 * SW + c0:2 * SW + c0 + wd], c == 0, c == 7,
                                (B_win, B_xn), (self.B_ps[pb],))
                    self.copy("act" if half == 0 else "dve", vst[vi][:, c0:c0 + wd], self.ps[pb][:, 0:wd], (self.B_ps[pb],), (B_vst[vi],))
                S.dma("pool", self.dsem_st[4 + vi], self.dmaf(self.vv[t0 + tt_ * 128:t0 + (tt_ + 1) * 128, :], vst[vi][:]), (B_vst[vi],), (self.B_dram["vv"],))
            if fox:
                pb = 6
                for c in range(NC8):
                    self.mm(self.ps[pb][0:NH, :], win[:, c, 3 * SW:3 * SW + NH], xn[:, c, :], c == 0, c == 7, (B_win, B_xn), (self.B_ps[pb],))
                self.act(gT[:, t0:t0 + 512], self.ps[pb][0:NH, :], AF.Exp, (self.B_ps[pb], self.B_const, B_gT), (B_gT,), bias=self.nbfg[:, j:j + 1], scale=-1.0)
                self.act(gT[:, t0:t0 + 512], gT[:, t0:t0 + 512], AF.Ln, (B_gT,), (B_gT,), bias=1.0, scale=1.0)
                init = 0.0 if I == 0 else gT[:, t0 - 1:t0]
                S.op("dve", (lambda o, d0, d1, ini: (lambda e: e.tensor_tensor_scan(out=o, data0=d0, data1=d1, initial=ini, op0=ALU.mult, op1=ALU.add)))(
                    gT[:, t0:t0 + 512], onesrow[:, :], gT[:, t0:t0 + 512], init), (B_gT, B_onesrow), (B_gT,))
        if fox:
            self.ts("dve", g8t[:], gT[:], -8.0, None, ALU.mult, None, (B_gT,), (B_g8t,))
            S.dma("pool", self.dsem_misc2, self.dmaf(self.g8[:, :], g8t[:]), (B_g8t,), (self.B_dram["g8"],))
            gview = self.gtab[:].rearrange("p (h j) -> p j h", h=NH)
            for j0 in range(0, NT, 32):
                nj = min(32, NT - j0)
                pb = (j0 // 32) % 2
                for jj in range(nj):
                    jt = j0 + jj
                    self.mm(self.ps[pb][:, jj * NH:(jj + 1) * NH], gT[:, jt * 128:(jt + 1) * 128], self.identf[0:NH, 0:NH], True, True,
                            (B_gT, self.B_const), (self.B_ps[pb],))
                self.copy("dve", gview[:, j0:j0 + nj, :], self.ps[pb][:, 0:nj * NH].rearrange("p (j h) -> p j h", h=NH), (self.B_ps[pb],), (self.B_gtab,))

    def phase_B(self, l):
        S = self.S
        SL, NT, NQ, NB = self.SL, self.NT, self.NQ, self.NB
        fox = (l % 2 == 0)
        R = "B"
        KA = 97
        LA = 2
        KT = [self.sb([128, SL], BF16, R) for _ in range(2)]
        QT = [self.sb([128, SL], BF16, R) for _ in range(2)]
        VA = [self.sb([128, NT, 72], BF16, R) for _ in range(2)]
        B_K = [Buf("K0"), Buf("K1")]
        B_Q = [Buf("Q0"), Buf("Q1")]
        B_V = [Buf("V0"), Buf("V1")]
        B_Qm = [[Buf("Qm%d_%d" % (s, I)) for I in range(NQ)] for s in range(2)]
        PT = [self.sb([128, 512], BF16, R) for _ in range(4)]
        B_PT = [Buf("PT%d" % i) for i in range(4)]
        rrow = [self.sb([128, 512], F32, R) for _ in range(4)]
        B_rrow = [Buf("rrow%d" % i) for i in range(4)]
        sbB = [self.sb([64, 512], F32, R) for _ in range(2)]
        B_sbB = [Buf("sbB0"), Buf("sbB1")]
        hst = [self.sb([64, 512], BF16, R) for _ in range(2)]
        B_hst = [Buf("hst0"), Buf("hst1")]
        zb = self.sb([128, 1], F32, R)
        B_zb = Buf("zb")
        self.memset("dve", zb[:], 0.0, (B_zb,))
        if not fox:
            gm = self.sb([128, 4, 32], F32, R)
            B_gm = Buf("gm")
            m8 = self.sb([128, 4, 8], F32, R)
            B_m8 = Buf("m8")
            thr = self.sb([128, 4], F32, R)
            B_thr = Buf("thr")
            stage = [self.sb([128, 96], BF16, R) for _ in range(4)]
            B_stage = [Buf("stage%d" % i) for i in range(4)]
            kms = self.sb([64, NB], F32, R)
            kmh = self.sb([64, NB], BF16, R)
            kml = self.sb([64, NB], BF16, R)
            kmhf = self.sb([64, NB], F32, R)
            B_km = Buf("km")
            akbh = [self.sb([128, NT], F32, R) for _ in range(2)]
            B_akbh = [Buf("akbh0"), Buf("akbh1")]
            for i in range(4):
                self.memset("dve", stage[i][:], 0.0, (B_stage[i],))
        for s in range(2):
            self.memset("dve", VA[s][:, :, 64:65], 1.0, (B_V[s],))
            self.memset("pool", KT[s][96:97, :], 1.0, (B_K[s],))
            if fox:
                self.memset("pool", QT[s][64:96, :], 0.0, (B_Q[s],))
        B_Kst = Buf("kstatic")
        S.dma_group("sp", self.dsem_misc, [(self.dmaf(KT[s_][64:96, :], self.c_onehot[:, :]), (), (B_Kst,)) for s_ in range(2)])
        psS = [0, 1, 2]
        psO = [3, 4]
        psBk = 5
        psG = 6
        psTr = 7
        heads = [("self", h) for h in range(NH)] + [("mem", h) for h in range(NMH)]

        def load_head(idx):
            kind, h = heads[idx]
            s = idx % 2
            items = []
            if kind == "self":
                items.append((self.dmaf(KT[s][0:64, :], self.kT[h * 64:(h + 1) * 64, :]), (self.B_dram["kT"],), (B_K[s],)))
                items.append((self.dmaf(QT[s][0:64, :], self.qT[h * 64:(h + 1) * 64, :]), (self.B_dram["qT"],), (B_Q[s],)))
                shift = self.g8[h:h + 1, :] if fox else self.c_aqs[h:h + 1, :]
                items.append((self.dmaf(QT[s][96:97, :], shift), (self.B_dram["g8"],), (B_Q[s],)))
                items.append((self.dmaf(VA[s][:, :, 0:64], self.vv[:, h * 64:(h + 1) * 64].rearrange("(j p) d -> p j d", p=128)),
                              (self.B_dram["vv"],), (B_V[s],)))
            else:
                items.append((self.dmaf(KT[s][0:64, 0:NMEM], self.mkT[h * 64:(h + 1) * 64, :]), (self.B_dram["mkT"],), (B_K[s],)))
                items.append((self.dmaf(QT[s][0:64, :], self.qmT[h * 64:(h + 1) * 64, :]), (self.B_dram["qmT"],), (B_Q[s],)))
                items.append((self.dmaf(VA[s][:, 0:2, 0:64], self.mv[:, h * 64:(h + 1) * 64].rearrange("(j p) d -> p j d", p=128)),
                              (self.B_dram["mv"],), (B_V[s],)))
            S.dma_group("sp", self.dsem_ld[s], items)

        def kmean(idx):
            s = idx % 2
            h = heads[idx][1]
            S.op("dve", (lambda o, i: (lambda e: e.tensor_reduce(out=o, in_=i, axis=AX.X, op=ALU.add)))(
                kms[:, :], KT[s][0:64, :].rearrange("d (n s) -> d n s", s=256)), (B_K[s],), (B_km,))
            self.ts("dve", kmh[:, :], kms[:, :], 1.0 / 256.0, None, ALU.mult, None, (B_km,), (B_km,))
            self.copy("dve", kmhf[:, :], kmh[:, :], (B_km,), (B_km,))
            S.op("dve", (lambda o, i0, i1: (lambda e: e.scalar_tensor_tensor(out=o, in0=i0, scalar=1.0 / 256.0, in1=i1, op0=ALU.mult, op1=ALU.subtract)))(
                kml[:, :], kms[:, :], kmhf[:, :]), (B_km,), (B_km,))
            self.ts("dve", akbh[s][:, :], self.akb[:, h * NT:(h + 1) * NT], 1.0, None, ALU.mult, None, (self.B_const,), (B_akbh[s],))

        def gate1(idx, I):
            s = idx % 2
            pg = self.ps[psG]
            B_pg = self.B_ps[psG]
            for qi in range(4):
                qt = 4 * I + qi
                self.mm(pg[:, qi * 32:qi * 32 + NB], QT[s][0:64, qt * 128:(qt + 1) * 128], kmh[:, :], True, False, (B_Q[s], B_km), (B_pg,))
                self.mm(pg[:, qi * 32:qi * 32 + NB], QT[s][0:64, qt * 128:(qt + 1) * 128], kml[:, :], False, True, (B_Q[s], B_km), (B_pg,))
            for qi in range(4):
                own = (4 * I + qi) // 2
                self.tt("dve", gm[:, qi, 0:NB], pg[:, qi * 32:qi * 32 + NB], self.bm[:, own * 32:own * 32 + NB], ALU.add, (B_pg, self.B_const), (B_gm,))
            for qi in range(4):
                S.op("dve", (lambda o, i: (lambda e: e.max(out=o, in_=i)))(m8[:, qi, :], gm[:, qi, 0:NB]), (B_gm,), (B_m8,))
            self.ts("dve", thr[:, :], m8[:, :, 3], -1e29, None, ALU.max, None, (B_m8,), (B_thr,))
            for qi in range(4):
                self.ts("dve", stage[qi][:, 64:64 + NB], gm[:, qi, 0:NB], thr[:, qi:qi + 1], MASKV, ALU.is_lt, ALU.mult, (B_gm, B_thr), (B_stage[qi],))

        def gate2(idx, I):
            s = idx % 2
            ptr = self.ps[psTr]
            B_ptr = self.B_ps[psTr]
            for qi in range(4):
                self.mm(ptr[0:96, qi * 128:(qi + 1) * 128], stage[qi][:, :], self.identb[:, :], True, True, (B_stage[qi], self.B_const), (B_ptr,))
            self.copy("act", QT[s][64:96, I * 512:(I + 1) * 512], ptr[64:96, :], (B_ptr,), (B_Qm[s][I],))

        units = []
        for idx, (kind, h) in enumerate(heads):
            k = 0
            for I in range(NQ):
                jl = list(range(4 * I + 4)) if kind == "self" else [0, 1]
                for ji, jt in enumerate(jl):
                    units.append((idx, I, ji, jt, len(jl), k))
                    k += 1
        n = len(units)
        state = {"step": 0}
        deferred = []

        def defer(k, fn, key=None):
            deferred.append((state["step"] + k, fn, key))

        def run_due(force=False, key=None):
            for ent in deferred[:]:
                due, fn, kk = ent
                if force or due <= state["step"] or (key is not None and kk == key):
                    deferred.remove(ent)
                    fn()

        def stage12(u, uid):
            idx, I, ji, jt, nj, k = u
            kind, h = heads[idx]
            s = idx % 2
            selfh = (kind == "self")
            moba = selfh and not fox
            K = KA if selfh else 64
            t0 = I * 512
            c0 = 0
            diag = False
            if selfh and jt >= 4 * I:
                c0 = 128 * (jt - 4 * I)
                diag = True
            sb_ = psS[uid % 3]
            pi_ = uid % 4
            pS = self.ps[sb_]
            rd = [B_K[s], B_Q[s], B_Kst]
            if moba:
                rd.append(B_Qm[s][I])
            self.mm(pS[:, c0:512], KT[s][0:K, jt * 128:(jt + 1) * 128], QT[s][0:K, t0 + c0:t0 + 512], True, not diag, rd, (self.B_ps[sb_],))
            if diag:
                self.mm(pS[:, c0:c0 + 128], self.identb[:, :], self.tri[:, :], False, True, (self.B_const,), (self.B_ps[sb_],))
            if not selfh:
                bias = zb[:, 0:1]
                rb = B_zb
            elif fox:
                bias = self.gtab[:, h * NT + jt:h * NT + jt + 1]
                rb = self.B_gtab
            else:
                bias = akbh[s][:, jt:jt + 1]
                rb = B_akbh[s]
            self.act(PT[pi_][:, c0:512], pS[:, c0:512], AF.Exp, (self.B_ps[sb_], rb), (B_PT[pi_],), bias=bias, scale=0.125)

        chunk_seq = {}

        def stage3(u, uid):
            idx, I, ji, jt, nj, k = u
            kind, h = heads[idx]
            s = idx % 2
            selfh = (kind == "self")
            t0 = I * 512
            c0 = 0
            if selfh and jt >= 4 * I:
                c0 = 128 * (jt - 4 * I)
            cs = idx * NQ + I
            if selfh:
                o = psO[cs % 2]
            else:
                o = (psO + [psG, psTr])[cs % 4]
            po = self.ps[o]
            B_po = self.B_ps[o]
            pi_ = uid % 4
            if ji == 0:
                run_due(key=("fin", o))
            self.mm(po[0:65, c0:512], VA[s][:, jt, 0:65], PT[pi_][:, c0:512], ji == 0, ji == nj - 1, (B_V[s], B_PT[pi_]), (B_po,))
            if k == 0 and idx + 1 < len(heads):
                load_head(idx + 1)
            if ji == nj - 1:
                hi = cs % 2
                ri = cs % 4
                S.op("dve", (lambda o_, i_: (lambda e: e.reciprocal(out=o_, in_=i_)))(rrow[ri][64:65, :], po[64:65, :]), (B_po,), (B_rrow[ri],))

                def fin(hi=hi, ri=ri, po=po, B_po=B_po, h=h, selfh=selfh, t0=t0):
                    pb_ = self.ps[psBk]
                    self.mm(pb_[0:64, :], self.onesf[64:65, 0:64], rrow[ri][64:65, :], True, True, (B_rrow[ri], self.B_const), (self.B_ps[psBk],))
                    self.copy("dve", sbB[hi][:, :], pb_[0:64, :], (self.B_ps[psBk],), (B_sbB[hi],))
                    self.tt("dve", hst[hi][:, :], po[0:64, :], sbB[hi][:, :], ALU.mult, (B_po, B_sbB[hi]), (B_hst[hi],))
                    row0 = h * 64 if selfh else SW + h * 64
                    S.dma("pool", self.dsem_st[hi], self.dmaf(self.hdT[row0:row0 + 64, t0:t0 + 512], hst[hi][:, :]), (B_hst[hi],), (self.B_dram["hdT"],))
                defer(12 if selfh else 3, fin, key=("fin", o))

        nhu = sum(4 * I + 4 for I in range(NQ))
        k_km = max(LA + 1, min(64, nhu // 8))
        k_g0 = k_km + max(1, min(16, nhu // 16))
        g_stride = max(1, min(24, (nhu - k_g0 - 12) // NQ))
        load_head(0)
        if not fox:
            kmean(0)
            for I in range(NQ):
                gate1(0, I)
                gate2(0, I)
        for step in range(n + LA):
            state["step"] = step
            if step < n:
                u = units[step]
                idx, I, ji, jt, nj, k = u
                if (not fox) and idx + 1 < NH:
                    if k == k_km:
                        kmean(idx + 1)
                    g, r = divmod(k - k_g0, g_stride)
                    if k >= k_g0 and r == 0 and g < NQ:
                        run_due(key="gate2")
                        gate1(idx + 1, g)
                        defer(10, (lambda a, b: (lambda: gate2(a, b)))(idx + 1, g), key="gate2")
                stage12(u, step)
            if step - LA >= 0:
                stage3(units[step - LA], step - LA)
            run_due()
        run_due(force=True)

    def phase_C(self, l):
        S = self.S
        SL = self.SL
        R = "C"
        N = 256
        NCH = SL // N
        last = (l == self.depth - 1)
        wo = self.sb([128, 8, D], BF16, R)
        wgu = self.sb([128, 8, 2 * DFF], BF16, R)
        wdn = self.sb([128, 22, D], BF16, R)
        B_wo, B_wgu, B_wdn = Buf("wo"), Buf("wgu"), Buf("wdn")
        stg = [(self.sb([128, 1024], F32, R), Buf("stgc%d" % i), self.dsem_w[i]) for i in range(2)]
        hch = [self.sb([128, 8, N], F32, R) for _ in range(2)]
        B_hch = [Buf("hc0"), Buf("hc1")]
        hdc1 = self.sb([128, 8, N], BF16, R)
        hdc = [hdc1, hdc1]
        B_hd1 = Buf("hd")
        B_hdc = [B_hd1, B_hd1]
        hn = self.sb([128, 8, N], BF16, R)
        B_hn = Buf("hn")
        sq = hn
        B_sq = B_hn
        rstd = self.sb([128, N], F32, R)
        B_rstd = Buf("rstdc")
        aT_off = self.off[R]
        aT = self.sb([128, 22, N], BF16, R)
        B_aT = Buf("aT")
        self.nt += 1
        stg_al = self.nc.alloc_sbuf_tensor_at("t%d" % self.nt, [128, 2, 1024], F32, offset=aT_off)
        stg_big = stg + [(stg_al[:, 0, :], Buf("stga0"), self.dsem_w[2]), (stg_al[:, 1, :], Buf("stga1"), self.dsem_w[3])]
        gact = [self.sb([128, N], F32, R) for _ in range(2)]
        B_gact = [Buf("ga%d" % i) for i in range(2)]
        hsrc = self.xT if l == 0 else self.hT
        self.load_weight(wo, lambda k: self.w_out[l, k * 128:(k + 1) * 128, :], D, 8, ["dve", "act", "dve", "act", "pool"], B_wo, stg_big, pw=1024)
        self.load_weight(wgu, lambda k: self.w_gu[l, k * 128:(k + 1) * 128, :], 2 * DFF, 8, ["dve", "act", "dve", "act", "pool"], B_wgu, stg_big, pw=1024)
        self.load_weight(wdn, lambda k: self.w_dn[l, k * 128:(k + 1) * 128, :], D, 22, ["pool", "dve", "act"], B_wdn, stg, pw=1024)

        def load_chunk(I):
            b = I % 2
            S.dma("sp", self.dsem_ld[b], self.dmaf(hch[b][:], hsrc[:, I * N:(I + 1) * N].rearrange("(c p) n -> p c n", p=128)),
                  (self.B_dram["hT"],), (B_hch[b],))

        def load_hd(I):
            b = I % 2
            S.dma("sp", self.dsem_ld[2 + b], self.dmaf(hdc[b][:], self.hdT[:, I * N:(I + 1) * N].rearrange("(c p) n -> p c n", p=128)),
                  (self.B_dram["hdT"],), (B_hdc[b],))
        load_chunk(0)
        load_hd(0)
        if NCH > 1:
            load_chunk(1)
        st_ = {"pi": 0, "ga": 0}

        def outproj(I):
            b = I % 2
            for fo in range(8):
                pb = st_["pi"] % 7
                st_["pi"] += 1
                for c in range(8):
                    self.mm(self.ps[pb][:, 0:N], wo[:, c, fo * 128:(fo + 1) * 128], hdc[b][:, c, :], c == 0, c == 7, (B_wo, B_hdc[b]), (self.B_ps[pb],))
                self.tt("dve", hch[b][:, fo, :], self.ps[pb][:, 0:N], hch[b][:, fo, :], ALU.add, (self.B_ps[pb], B_hch[b]), (B_hch[b],))
            if I + 1 < NCH:
                load_hd(I + 1)

        def norm(I):
            b = I % 2
            self.rmsnorm(hch[b], B_hch[b], lambda c: self.gffn[:, l * 8 + c:l * 8 + c + 1], N, hn, B_hn, sq, B_sq, 7, rstd, B_rstd)

        def down(I, fos):
            b = I % 2
            for fo in fos:
                pb = st_["pi"] % 7
                st_["pi"] += 1
                for k in range(22):
                    self.mm(self.ps[pb][:, 0:N], wdn[:, k, fo * 128:(fo + 1) * 128], aT[:, k, :], k == 0, k == 21, (B_wdn, B_aT), (self.B_ps[pb],))
                self.tt("dve", hch[b][:, fo, :], self.ps[pb][:, 0:N], hch[b][:, fo, :], ALU.add, (self.B_ps[pb], B_hch[b]), (B_hch[b],))

        outproj(0)
        norm(0)
        for I in range(NCH):
            b = I % 2
            t0 = I * N
            for ft in range(22):
                pg = st_["pi"] % 7
                st_["pi"] += 1
                for c in range(8):
                    self.mm(self.ps[pg][:, 0:N], wgu[:, c, ft * 128:(ft + 1) * 128], hn[:, c, :], c == 0, c == 7, (B_wgu, B_hn), (self.B_ps[pg],))
                gi = st_["ga"] % 2
                st_["ga"] += 1
                self.act(gact[gi][:, :], self.ps[pg][:, 0:N], AF.Silu, (self.B_ps[pg],), (B_gact[gi],))
                pu = st_["pi"] % 7
                st_["pi"] += 1
                for c in range(8):
                    self.mm(self.ps[pu][:, 0:N], wgu[:, c, DFF + ft * 128:DFF + (ft + 1) * 128], hn[:, c, :], c == 0, c == 7, (B_wgu, B_hn), (self.B_ps[pu],))
                self.tt("dve", aT[:, ft, :], self.ps[pu][:, 0:N], gact[gi][:, :], ALU.mult, (self.B_ps[pu], B_gact[gi]), (B_aT,))
            if I + 1 < NCH:
                outproj(I + 1)
            down(I, range(0, 4))
            if I + 1 < NCH:
                norm(I + 1)
            down(I, range(4, 8))
            if not last:
                S.dma("pool", self.dsem_st[b], self.dmaf(self.hT[:, t0:t0 + N].rearrange("(c p) n -> p c n", p=128), hch[b][:]),
                      (B_hch[b],), (self.B_dram["hT"],))
            else:
                self.final_norm(hch[b], B_hch[b], N, aT, B_aT, rstd, B_rstd)
                S.dma("pool", self.dsem_st[b], self.dmaf(self.outT[:, t0:t0 + N].rearrange("(c p) n -> p c n", p=128), hch[b][:]),
                      (B_hch[b],), (self.B_dram["outT"],))
            if I + 2 < NCH:
                load_chunk(I + 2)

    def final_norm(self, hch, B_h, N, sq, B_sq, rstd, B_rstd):
        ps_t = self.ps[7]
        B_p = self.B_ps[7]
        for c in range(NC8):
            self.act(sq[:, c, 0:N], hch[:, c, 0:N], AF.Square, (B_h,), (B_sq,))
        for c in range(NC8):
            self.mm(ps_t[:, 0:N], self.onesm[:], sq[:, c, 0:N], c == 0, c == NC8 - 1, (B_sq, self.B_const), (B_p,))
        self.act(rstd[:, 0:N], ps_t[:, 0:N], AF.Ln, (B_p, self.B_const), (B_rstd,), bias=self.epsb[:, 0:1], scale=1.0)
        self.act(rstd[:, 0:N], rstd[:, 0:N], AF.Exp, (B_rstd,), (B_rstd,), scale=-0.5)
        for c in range(NC8):
            self.S.op("dve", (lambda o, i0, sc, i1: (lambda e: e.scalar_tensor_tensor(out=o, in0=i0, scalar=sc, in1=i1, op0=ALU.mult, op1=ALU.mult)))(
                hch[:, c, 0:N], hch[:, c, 0:N], self.gfin[:, c:c + 1], rstd[:, 0:N]), (B_h, B_rstd, self.B_const), (B_h,))


def _consts(SL):
    NT = SL // 128
    NB = SL // 256
    bf = ml_dtypes.bfloat16
    c = {}
    c["c_identb"] = np.eye(128, dtype=np.float32).astype(bf)
    c["c_identf"] = np.eye(128, dtype=np.float32)
    s = np.arange(128)
    c["c_tri"] = np.where(s[None, :] >= s[:, None], 0.0, MASKV).astype(np.float32).astype(bf)
    pos = np.arange(SL)
    c["c_onehot"] = (pos[None, :] // 256 == np.arange(32)[:, None]).astype(np.float32).astype(bf)
    slopes = (2.0 ** (-8.0 * np.arange(1, NH + 1, dtype=np.float32) / NH)).astype(np.float32)
    p = np.arange(128, dtype=np.float32)
    jj = np.arange(NT, dtype=np.float32)
    kb = slopes[None, :, None] * (128.0 * jj[None, None, :] + p[:, None, None])
    c["c_alibi_kbias"] = kb.reshape(128, NH * NT).astype(np.float32)
    c["c_alibi_qshift"] = (-8.0 * slopes[:, None] * pos[None, :].astype(np.float32)).astype(np.float32).astype(bf)
    bm = np.zeros((NB, 32), np.float32)
    for own in range(NB):
        bm[own, own] = 1e30
        bm[own, own + 1:] = -1e30
    c["c_moba_biasmask"] = np.ascontiguousarray(np.broadcast_to(bm.reshape(1, NB * 32), (128, NB * 32))).astype(np.float32)
    return c


def _gain_layout(g):
    L = g.shape[0]
    return np.ascontiguousarray(g.reshape(L, 8, 128).transpose(2, 0, 1).reshape(128, L * 8)).astype(np.float32)


_CACHE = {}


def run(x, mem, norm_mix, norm_mem, norm_ffn, norm_final, w_in_fox, b_fgate, w_in_moba, w_mem_kv, w_out,
        w_gate_up, w_down, n_cores=8):
    B, SL, _ = x.shape
    depth = norm_mix.shape[0]
    key = (SL, depth)
    if key not in _CACHE:
        _CACHE[key] = Prog(SL, depth).build()
    nc = _CACHE[key]
    consts = _consts(SL)
    shared = dict(consts)
    shared["g_mix"] = _gain_layout(np.asarray(norm_mix))
    shared["g_mem"] = _gain_layout(np.asarray(norm_mem))
    shared["g_ffn"] = _gain_layout(np.asarray(norm_ffn))
    shared["g_fin"] = _gain_layout(np.asarray(norm_final)[None, :])
    shared["w_in_fox"] = np.ascontiguousarray(w_in_fox, dtype=np.float32)
    shared["b_fgate"] = np.ascontiguousarray(np.asarray(b_fgate).T, dtype=np.float32)
    wm = np.asarray(w_in_moba, dtype=np.float32)
    if wm.shape[0] == 0:
        wm = np.zeros((1, D, MOBAP), np.float32)
    shared["w_in_moba"] = np.ascontiguousarray(wm)
    shared["w_mem_kv"] = np.ascontiguousarray(w_mem_kv, dtype=np.float32)
    shared["w_out"] = np.ascontiguousarray(w_out, dtype=np.float32)
    shared["w_gate_up"] = np.ascontiguousarray(w_gate_up, dtype=np.float32)
    shared["w_down"] = np.ascontiguousarray(w_down, dtype=np.float32)
    work = [0, 1, 4, 5][:B] if n_cores == 8 else list(range(B))
    idle = None
    in_maps = []
    for cidx in range(n_cores):
        if cidx in work:
            b = work.index(cidx)
            m = dict(shared)
            m["xT"] = np.ascontiguousarray(np.asarray(x[b]).T, dtype=np.float32)
            m["memT"] = np.ascontiguousarray(np.asarray(mem[b]).T, dtype=np.float32)
        else:
            if idle is None:
                idle = {k: (v if k.startswith("c_") else np.zeros_like(v)) for k, v in shared.items()}
                idle["xT"] = np.zeros((D, SL), np.float32)
                idle["memT"] = np.zeros((D, NMEM), np.float32)
            m = idle
        in_maps.append(m)
    res = run_bass_kernel_spmd(nc, in_maps, core_ids=list(range(n_cores)))
    out = np.empty((B, SL, D), np.float32)
    for b in range(B):
        out[b] = res.results[work[b]]["outT"].T
    return out


def kernel(x, mem, norm_mix, norm_mem, norm_ffn, norm_final, w_in_fox, b_fgate, w_in_moba, w_mem_kv, w_out,
           w_gate_up, w_down):
    return run(x, mem, norm_mix, norm_mem, norm_ffn, norm_final, w_in_fox, b_fgate, w_in_moba, w_mem_kv, w_out,
               w_gate_up, w_down, n_cores=8)
```
